# Optimizing a Trainium2 kernel written in Bass

```python
import math
import jax
import jax.numpy as jnp
from jax import lax
import numpy as np

D_MODEL = 1024
BATCH = 4
SEQ = 8192
DEPTH = 2

A_WIDTH = 512
A_CONV = 3
B_WIDTH = 512
B_CONV = 31
C_HEADS = 8
C_HEAD_DIM = 64
IDX_HEADS = 8
IDX_DIM = 32
TOPK_MAX = 256
DIFF_HEADS = 4
DIFF_HEAD_DIM = 64
NUM_BUCKETS = 32
MAX_DISTANCE = 128
N_ATTN_HEADS = C_HEADS + DIFF_HEADS
N_GROUPS = 4
EXPERTS_PER_GROUP = 8
N_EXPERTS = N_GROUPS * EXPERTS_PER_GROUP
TOP_E = 2
D_EXPERT = 512
Q_BLOCK = 128
MOE_BLOCK = 128
EPS = 1e-6
N_EVEN = (DEPTH + 1) // 2
N_ODD = DEPTH // 2

AB_SIZES = (A_WIDTH, A_WIDTH, A_WIDTH, B_WIDTH, B_WIDTH)
AB_IN = 3 * A_WIDTH + 2 * B_WIDTH
C_WIDTH = C_HEADS * C_HEAD_DIM
DIFF_QK = DIFF_HEADS * 2 * DIFF_HEAD_DIM
DIFF_V = DIFF_HEADS * 2 * DIFF_HEAD_DIM
CD_SIZES = (C_WIDTH, C_WIDTH, C_WIDTH, IDX_HEADS * IDX_DIM, IDX_DIM, IDX_HEADS, DIFF_QK, DIFF_QK, DIFF_V)
CD_IN = 3 * C_WIDTH + IDX_HEADS * IDX_DIM + IDX_DIM + IDX_HEADS + 2 * DIFF_QK + DIFF_V

kernel_name = 'hybrid_conv_sparse_diff_hmoe_trunk'


def rmsnorm(x, g):
    xf = x.astype(jnp.float32)
    y = xf * lax.rsqrt(jnp.mean(xf * xf, axis=-1, keepdims=True) + EPS)
    return (y * g.astype(jnp.float32)).astype(x.dtype)


def layernorm(x, g, b):
    xf = x.astype(jnp.float32)
    mu = jnp.mean(xf, axis=-1, keepdims=True)
    var = jnp.mean(jnp.square(xf - mu), axis=-1, keepdims=True)
    y = (xf - mu) * lax.rsqrt(var + EPS) * g.astype(jnp.float32) + b.astype(jnp.float32)
    return y.astype(x.dtype)


def modulate(h, shift, scale):
    return h * (1 + scale[:, None, :]) + shift[:, None, :]


def split_cols(z, sizes):
    cuts = [int(v) for v in np.cumsum(sizes)[:-1]]
    return jnp.split(z, cuts, axis=-1)


def causal_depthwise_conv(u, w):
    width, ch = w.shape
    return lax.conv_general_dilated(u, w[:, None, :].astype(u.dtype), window_strides=(1,),
                                    padding=[(width - 1, 0)],
                                    dimension_numbers=('NWC', 'WIO', 'NWC'),
                                    feature_group_count=ch)


def rel_bucket(dist):
    max_exact = NUM_BUCKETS // 2
    n = jnp.maximum(dist, 0)
    log_ratio = jnp.log(jnp.maximum(n, 1).astype(jnp.float32) / max_exact) / math.log(MAX_DISTANCE / max_exact)
    large = max_exact + (log_ratio * (NUM_BUCKETS - max_exact)).astype(jnp.int32)
    return jnp.where(n < max_exact, n, jnp.minimum(large, NUM_BUCKETS - 1))


def conv_mixers(h, w_in, conv_a, conv_b, conv_b_bias, ln_g, ln_b, w_out):
    gate_b, gate_c, x_a, val_b, glu_gate = split_cols(h @ w_in, AB_SIZES)
    y_a = gate_b * causal_depthwise_conv(gate_c * x_a, conv_a)
    u = causal_depthwise_conv(val_b * jax.nn.sigmoid(glu_gate), conv_b) + conv_b_bias
    y_b = jax.nn.silu(layernorm(u, ln_g, ln_b))
    return jnp.concatenate([y_a, y_b], axis=-1) @ w_out


def attn_mixers(h, positions, w_in, rel_bias, diff_lam, diff_norm_g, w_out, lambda_init):
    bsz, seq, _ = h.shape
    n_blocks = seq // Q_BLOCK
    topk = min(TOPK_MAX, seq // 4)
    q_c, k_c, v_c, q_idx, k_idx, w_idx, q_d, k_d, v_d = split_cols(h @ w_in, CD_SIZES)
    q_c = q_c.reshape(bsz, seq, C_HEADS, C_HEAD_DIM)
    k_c = k_c.reshape(bsz, seq, C_HEADS, C_HEAD_DIM)
    v_c = v_c.reshape(bsz, seq, C_HEADS, C_HEAD_DIM)
    q_idx = q_idx.reshape(bsz, seq, IDX_HEADS, IDX_DIM)
    q_d = q_d.reshape(bsz, seq, DIFF_HEADS, 2, DIFF_HEAD_DIM)
    k_d = k_d.reshape(bsz, seq, DIFF_HEADS, 2, DIFF_HEAD_DIM)
    q1, q2 = q_d[..., 0, :], q_d[..., 1, :]
    k1, k2 = k_d[..., 0, :], k_d[..., 1, :]
    v_d = v_d.reshape(bsz, seq, DIFF_HEADS, 2 * DIFF_HEAD_DIM)
    lam_f = diff_lam.astype(jnp.float32)
    lam = jnp.exp(jnp.sum(lam_f[0] * lam_f[1])) - jnp.exp(jnp.sum(lam_f[2] * lam_f[3])) + lambda_init
    bias_c = rel_bias[:, :C_HEADS]
    bias_d = rel_bias[:, C_HEADS:]
    scale_c = C_HEAD_DIM ** -0.5
    scale_d = DIFF_HEAD_DIM ** -0.5
    scale_idx = (IDX_DIM * IDX_HEADS) ** -0.5
    gather_rows = jax.vmap(lambda a, i: a[i])

    def block(j):
        s0 = j * Q_BLOCK
        take = lambda a: lax.dynamic_slice_in_dim(a, s0, Q_BLOCK, axis=1)
        pos_q = take(positions)
        causal = positions[:, None, :] <= pos_q[:, :, None]
        rel = jnp.einsum('bqhd,bsd->bqhs', take(q_idx), k_idx)
        score = jnp.einsum('bqhs,bqh->bqs', jax.nn.relu(rel), take(w_idx)).astype(jnp.float32) * scale_idx
        score = jnp.where(causal, score, -jnp.inf)
        _, sel = lax.top_k(score, topk)
        k_sel = gather_rows(k_c, sel)
        v_sel = gather_rows(v_c, sel)
        pos_sel = gather_rows(positions, sel)
        dist_c = pos_q[:, :, None] - pos_sel
        logit_c = (jnp.einsum('bqhd,bqkhd->bhqk', take(q_c), k_sel).astype(jnp.float32) * scale_c
                   + jnp.moveaxis(bias_c[rel_bucket(dist_c)], -1, 1))
        logit_c = jnp.where((dist_c >= 0)[:, None], logit_c, -jnp.inf)
        p_c = jax.nn.softmax(logit_c, axis=-1).astype(v_sel.dtype)
        out_c = jnp.einsum('bhqk,bqkhd->bqhd', p_c, v_sel)
        dist_d = pos_q[:, :, None] - positions[:, None, :]
        b_d = jnp.moveaxis(bias_d[rel_bucket(dist_d)], -1, 1)
        mask_d = causal[:, None]

        def softmax_map(qb, kk):
            lg = jnp.einsum('bqhd,bshd->bhqs', qb, kk).astype(jnp.float32) * scale_d + b_d
            return jax.nn.softmax(jnp.where(mask_d, lg, -jnp.inf), axis=-1)

        attn = softmax_map(take(q1), k1) - lam * softmax_map(take(q2), k2)
        out_d = jnp.einsum('bhqs,bshd->bqhd', attn.astype(v_d.dtype), v_d)
        return out_c, out_d

    out_c, out_d = lax.map(block, jnp.arange(n_blocks))
    out_c = jnp.moveaxis(out_c, 0, 1).reshape(bsz, seq, C_WIDTH)
    out_d = jnp.moveaxis(out_d, 0, 1).reshape(bsz, seq, DIFF_HEADS, 2 * DIFF_HEAD_DIM)
    out_d = (rmsnorm(out_d, diff_norm_g) * (1 - lambda_init)).reshape(bsz, seq, DIFF_V)
    return jnp.concatenate([out_c, out_d], axis=-1) @ w_out


def hier_moe(h, wr_g, br_g, wr_e, br_e, w_gate, w_up, w_down):
    bsz, seq, d = h.shape
    n_tok = bsz * seq
    xf = h.reshape(n_tok, d)
    p_group = jax.nn.softmax((xf @ wr_g + br_g).astype(jnp.float32), axis=-1)
    p_top, g_sel = lax.top_k(p_group, 1)
    fine = (xf @ wr_e + br_e).astype(jnp.float32).reshape(n_tok, N_GROUPS, EXPERTS_PER_GROUP)
    fine = jnp.take_along_axis(fine, g_sel[:, :, None], axis=1)[:, 0]
    e_val, e_sel = lax.top_k(fine, TOP_E)
    gate = (p_top * jax.nn.softmax(e_val, axis=-1)).reshape(-1)
    expert_id = (g_sel * EXPERTS_PER_GROUP + e_sel).reshape(-1)
    token_id = jnp.repeat(jnp.arange(n_tok, dtype=jnp.int32), TOP_E)
    n_assign = n_tok * TOP_E
    order = jnp.argsort(expert_id)
    e_sorted = expert_id[order]
    counts = jnp.bincount(expert_id, length=N_EXPERTS)
    starts = jnp.cumsum(counts) - counts
    padded = (counts + MOE_BLOCK - 1) // MOE_BLOCK * MOE_BLOCK
    pad_end = jnp.cumsum(padded)
    pad_start = pad_end - padded
    dest = pad_start[e_sorted] + jnp.arange(n_assign, dtype=jnp.int32) - starts[e_sorted]
    n_blocks = -(-n_assign // MOE_BLOCK) + N_EXPERTS
    n_rows = n_blocks * MOE_BLOCK
    row_token = jnp.full((n_rows,), n_tok, jnp.int32).at[dest].set(token_id[order])
    row_gate = jnp.zeros((n_rows,), gate.dtype).at[dest].set(gate[order])
    block_expert = jnp.minimum(jnp.searchsorted(pad_end, jnp.arange(n_blocks, dtype=jnp.int32) * MOE_BLOCK, side='right'),
                               N_EXPERTS - 1)
    x_rows = jnp.concatenate([xf, jnp.zeros((1, d), xf.dtype)], axis=0)[row_token].reshape(n_blocks, MOE_BLOCK, d)

    def expert_block(args):
        xb, e = args
        return (jax.nn.silu(xb @ w_gate[e]) * (xb @ w_up[e])) @ w_down[e]

    y_rows = lax.map(expert_block, (x_rows, block_expert)).reshape(n_rows, d)
    y_rows = y_rows * row_gate[:, None].astype(y_rows.dtype)
    out = jnp.zeros((n_tok + 1, d), y_rows.dtype).at[row_token].add(y_rows)[:n_tok]
    return out.reshape(bsz, seq, d)


def setup_inputs(seed: int = 0) -> dict:
    key = jax.random.key(seed)
    ks = jax.random.split(key, 32)
    f32 = jnp.float32

    def nrm(k, shape, scale):
        return jax.random.normal(k, shape, f32) * scale

    return {
        'x': nrm(ks[0], (BATCH, SEQ, D_MODEL), 1.0),
        'c': nrm(ks[1], (BATCH, D_MODEL), 1.0),
        'positions': jnp.broadcast_to(jnp.arange(SEQ, dtype=jnp.int32), (BATCH, SEQ)),
        'rel_bias': nrm(ks[2], (NUM_BUCKETS, N_ATTN_HEADS), 0.5),
        'norm_g': 1.0 + nrm(ks[3], (DEPTH, 2, D_MODEL), 0.1),
        'final_norm_g': 1.0 + nrm(ks[4], (D_MODEL,), 0.1),
        'ada_w': nrm(ks[5], (DEPTH, D_MODEL, 6 * D_MODEL), 0.5 * D_MODEL ** -0.5),
        'ada_b': nrm(ks[6], (DEPTH, 6 * D_MODEL), 0.02),
        'ab_w_in': nrm(ks[7], (N_EVEN, D_MODEL, AB_IN), D_MODEL ** -0.5),
        'ab_conv_a': nrm(ks[8], (N_EVEN, A_CONV, A_WIDTH), A_CONV ** -0.5),
        'ab_conv_b': nrm(ks[9], (N_EVEN, B_CONV, B_WIDTH), B_CONV ** -0.5),
        'ab_conv_b_bias': nrm(ks[10], (N_EVEN, B_WIDTH), 0.02),
        'ab_ln_g': 1.0 + nrm(ks[11], (N_EVEN, B_WIDTH), 0.1),
        'ab_ln_b': nrm(ks[12], (N_EVEN, B_WIDTH), 0.02),
        'ab_w_out': nrm(ks[13], (N_EVEN, A_WIDTH + B_WIDTH, D_MODEL), (A_WIDTH + B_WIDTH) ** -0.5),
        'cd_w_in': nrm(ks[14], (N_ODD, D_MODEL, CD_IN), D_MODEL ** -0.5),
        'diff_lam': nrm(ks[15], (N_ODD, 4, DIFF_HEAD_DIM), 0.1),
        'diff_norm_g': 1.0 + nrm(ks[16], (N_ODD, 2 * DIFF_HEAD_DIM), 0.1),
        'cd_w_out': nrm(ks[17], (N_ODD, C_WIDTH + DIFF_V, D_MODEL), (C_WIDTH + DIFF_V) ** -0.5),
        'moe_wr_g': nrm(ks[18], (DEPTH, D_MODEL, N_GROUPS), D_MODEL ** -0.5),
        'moe_br_g': nrm(ks[19], (DEPTH, N_GROUPS), 0.01),
        'moe_wr_e': nrm(ks[20], (DEPTH, D_MODEL, N_EXPERTS), D_MODEL ** -0.5),
        'moe_br_e': nrm(ks[21], (DEPTH, N_EXPERTS), 0.01),
        'moe_w_gate': nrm(ks[22], (DEPTH, N_EXPERTS, D_MODEL, D_EXPERT), D_MODEL ** -0.5),
        'moe_w_up': nrm(ks[23], (DEPTH, N_EXPERTS, D_MODEL, D_EXPERT), D_MODEL ** -0.5),
        'moe_w_down': nrm(ks[24], (DEPTH, N_EXPERTS, D_EXPERT, D_MODEL), D_EXPERT ** -0.5),
    }


def reference(x, c, positions, rel_bias, norm_g, final_norm_g, ada_w, ada_b,
              ab_w_in, ab_conv_a, ab_conv_b, ab_conv_b_bias, ab_ln_g, ab_ln_b, ab_w_out,
              cd_w_in, diff_lam, diff_norm_g, cd_w_out,
              moe_wr_g, moe_br_g, moe_wr_e, moe_br_e, moe_w_gate, moe_w_up, moe_w_down):
    cond = jax.nn.silu(c)
    for i in range(DEPTH):
        mod = cond @ ada_w[i] + ada_b[i]
        sh1, sc1, g1, sh2, sc2, g2 = jnp.split(mod, 6, axis=-1)
        h = modulate(rmsnorm(x, norm_g[i, 0]), sh1, sc1)
        j = i // 2
        if i % 2 == 0:
            y = conv_mixers(h, ab_w_in[j], ab_conv_a[j], ab_conv_b[j], ab_conv_b_bias[j],
                            ab_ln_g[j], ab_ln_b[j], ab_w_out[j])
        else:
            lambda_init = 0.8 - 0.6 * math.exp(-0.3 * i)
            y = attn_mixers(h, positions, cd_w_in[j], rel_bias, diff_lam[j], diff_norm_g[j],
                            cd_w_out[j], lambda_init)
        x = x + g1[:, None, :] * y
        h = modulate(rmsnorm(x, norm_g[i, 1]), sh2, sc2)
        x = x + g2[:, None, :] * hier_moe(h, moe_wr_g[i], moe_br_g[i], moe_wr_e[i], moe_br_e[i],
                                          moe_w_gate[i], moe_w_up[i], moe_w_down[i])
    return rmsnorm(x, final_norm_g)
```

```python
import numpy as np
from contextlib import ExitStack
import concourse.bass as bass
import concourse.mybir as mybir

F32 = mybir.dt.float32
BF16 = mybir.dt.bfloat16
I32 = mybir.dt.int32
ALU = mybir.AluOpType
AF = mybir.ActivationFunctionType
AX = mybir.AxisListType


class Dep:
    __slots__ = ("w", "r", "name")

    def __init__(self, name=""):
        self.w = None
        self.r = {}
        self.name = name


class Sch:
    ENGS = ("pe", "act", "dve", "pool", "sp")

    def __init__(self, nc, es, name):
        self.nc = nc
        self.es = es
        self.name = name
        self.q = {e: [] for e in self.ENGS}
        self.sem = {e: es.enter_context(nc.semaphore(f"{name}_s_{e}")) for e in self.ENGS}
        self.cnt = {e: 0 for e in self.ENGS}
        self.seen = {e: {} for e in self.ENGS}
        self.dsem = {}
        self.dcnt = {}
        self.nd = 0

    def sb(self, shape, dt, name):
        return self.es.enter_context(self.nc.sbuf_tensor(f"{self.name}_{name}", list(shape), dt))

    def ps(self, shape, dt, name):
        return self.es.enter_context(self.nc.psum_tensor(f"{self.name}_{name}", list(shape), dt))

    def dma_stream(self, name):
        k = f"d{self.nd}_{name}"
        self.nd += 1
        self.dsem[k] = self.es.enter_context(self.nc.semaphore(f"{self.name}_{k}"))
        self.dcnt[k] = 0
        return k

    def _semof(self, key):
        return self.sem[key] if key in self.sem else self.dsem[key]

    def _waits(self, eng, reads, writes):
        need = {}

        def add(kc):
            if kc is None:
                return
            k, c = kc
            if c > need.get(k, 0):
                need[k] = c
        for d in reads:
            add(d.w)
        for d in writes:
            add(d.w)
            for k, c in d.r.items():
                add((k, c))
        out = []
        for k, c in need.items():
            if k == eng and eng == "pe":
                continue
            if self.seen[eng].get(k, 0) >= c:
                continue
            self.seen[eng][k] = c
            out.append((k, c))
        return out

    def op(self, eng, fn, reads=(), writes=()):
        ws = self._waits(eng, reads, writes)
        self.cnt[eng] += 1
        n = self.cnt[eng]
        self.q[eng].append((ws, fn, eng, 1))
        for d in reads:
            if d.r.get(eng, 0) < n:
                d.r[eng] = n
        for d in writes:
            d.w = (eng, n)
            d.r = {}
        return n

    def dma(self, queue, stream, out, in_, reads=(), writes=(), **kw):
        ws = self._waits(queue, reads, writes)
        self.dcnt[stream] += 16
        n = self.dcnt[stream]
        self.q[queue].append((ws, (lambda e, o=out, i=in_, k=kw: e.dma_start(out=o, in_=i, **k)), stream, 16))
        for d in reads:
            if d.r.get(stream, 0) < n:
                d.r[stream] = n
        for d in writes:
            d.w = (stream, n)
            d.r = {}
        return n

    def dmaop(self, queue, stream, fn, reads=(), writes=()):
        ws = self._waits(queue, reads, writes)
        self.dcnt[stream] += 16
        n = self.dcnt[stream]
        self.q[queue].append((ws, fn, stream, 16))
        for d in reads:
            if d.r.get(stream, 0) < n:
                d.r[stream] = n
        for d in writes:
            d.w = (stream, n)
            d.r = {}
        return n

    def finish(self, final_deps=()):
        need = {}
        for d in final_deps:
            if d.w:
                k, c = d.w
                need[k] = max(need.get(k, 0), c)
        fin = list(need.items())
        nc = self.nc
        with nc.Block() as block:
            allfin = [(k, c) for k, c in list(self.cnt.items()) + list(self.dcnt.items()) if c > 0]

            def run(e, eng):
                for ws, fn, key, inc in self.q[eng]:
                    for k, c in ws:
                        e.wait_ge(self._semof(k), c)
                    fn(e).then_inc(self._semof(key), inc)
                for k, c in allfin:
                    if k != eng:
                        e.wait_ge(self._semof(k), c)

            @block.tensor
            def _(e):
                run(e, "pe")

            @block.scalar
            def _(e):
                run(e, "act")

            @block.vector
            def _(e):
                run(e, "dve")

            @block.gpsimd
            def _(e):
                run(e, "pool")

            @block.sync
            def _(e):
                run(e, "sp")

    def mm(self, out, lhsT, rhs, start, stop, reads=(), writes=()):
        return self.op("pe", lambda e: e.matmul(out, lhsT, rhs, start=start, stop=stop), reads, writes)

    def tr(self, out, in_, ident, reads=(), writes=()):
        return self.op("pe", lambda e: e.transpose(out, in_, ident), reads, writes)

    def tt(self, eng, out, in0, in1, op, reads=(), writes=()):
        return self.op(eng, lambda e: e.tensor_tensor(out, in0, in1, op), reads, writes)

    def ts(self, eng, out, in0, s1, s2, op0, op1=None, reads=(), writes=(), accum_out=None):
        if accum_out is not None:
            return self.op(eng, lambda e: e.tensor_scalar(out, in0, s1, s2, op0=op0, op1=op1, accum_out=accum_out), reads, writes)
        if op1 is None:
            return self.op(eng, lambda e: e.tensor_scalar(out, in0, s1, None, op0=op0), reads, writes)
        return self.op(eng, lambda e: e.tensor_scalar(out, in0, s1, s2, op0=op0, op1=op1), reads, writes)

    def stt(self, out, in0, scalar, in1, op0, op1, reads=(), writes=()):
        return self.op("dve", lambda e: e.scalar_tensor_tensor(out, in0, scalar, in1, op0=op0, op1=op1), reads, writes)

    def act(self, out, in_, func, bias=None, scale=None, accum_out=None, reads=(), writes=()):
        kw = {}
        if bias is not None:
            kw["bias"] = bias
        if scale is not None:
            kw["scale"] = scale
        if accum_out is not None:
            kw["accum_out"] = accum_out
        return self.op("act", lambda e: e.activation(out, in_, func, **kw), reads, writes)

    def cp(self, eng, out, in_, reads=(), writes=()):
        if eng == "act":
            return self.op("act", lambda e: e.copy(out, in_), reads, writes)
        return self.op(eng, lambda e: e.tensor_copy(out, in_), reads, writes)

    def memset(self, eng, ap, val, writes=()):
        return self.op(eng, lambda e: e.memset(ap, val), (), writes)


class Ring:
    def __init__(self, S, n, shape, dt, name, psum=False):
        self.t = [(S.ps(shape, dt, f"{name}{i}") if psum else S.sb(shape, dt, f"{name}{i}")) for i in range(n)]
        self.d = [Dep(f"{name}{i}") for i in range(n)]
        self.i = 0
        self.n = n

    def next(self):
        k = self.i % self.n
        self.i += 1
        return self.t[k], self.d[k]


D = 1024
EPS = 1e-6


def bcast_row(ap_row, n):
    return ap_row.to_broadcast([128, n])


def load_consts(S, C):
    st = S.dma_stream("const")
    K = {}
    K["idb"] = S.sb([128, 128], BF16, "idb")
    K["idf"] = S.sb([128, 128], F32, "idf")
    K["onesf"] = S.sb([128, 128], F32, "onesf")
    K["dep"] = Dep("const")
    S.dma("sp", st, K["idb"][:], C["idb"], writes=[K["dep"]])
    S.dma("sp", st, K["idf"][:], C["idf"], writes=[K["dep"]])
    S.memset("pool", K["onesf"][:], 1.0, writes=[K["dep"]])
    return K


def phase_mod(nc, C, layer, name):
    with ExitStack() as es:
        S = Sch(nc, es, name)
        st = S.dma_stream("ld")
        so = S.dma_stream("st")
        carr = S.sb([128, 8], F32, "carr")
        dcarr = Dep()
        S.dma("sp", st, carr[:], C["c_arr"], writes=[dcarr])
        cond = S.sb([128, 8], F32, "cond")
        S.act(cond[:], carr[:], AF.Silu, reads=[dcarr], writes=[dcarr])
        CB = S.sb([128, 8, 128], F32, "CB")
        dCB = Dep()
        for kc in range(8):
            S.cp("dve", CB[:, kc, :], cond[:, kc:kc + 1].to_broadcast([128, 128]), reads=[dcarr], writes=[dCB])
        gb = S.sb([128, 2, 1024], F32, "gb")
        dgb = Dep()
        for j in range(2):
            S.dma("sp", st, gb[:, j, :], bcast_row(C["norm_g"][layer, j:j + 1, :], 1024), writes=[dgb])
        bb = S.sb([128, 6144], F32, "bb")
        dbb = Dep()
        S.dma("sp", st, bb[:], bcast_row(C["ada_b"][layer:layer + 1, :], 6144), writes=[dbb])
        wr = Ring(S, 2, [128, 8, 512], F32, "w")
        wst = [S.dma_stream("w0"), S.dma_stream("w1")]
        pr = Ring(S, 2, [128, 512], F32, "p", psum=True)
        orr = Ring(S, 2, [128, 512], F32, "o")
        row_of = {0: 1, 1: 0, 2: 2, 3: 4, 4: 3, 5: 5}
        dout = Dep()
        for blk in range(12):
            w, dw = wr.next()
            S.dma("sp", wst[blk % 2], w[:], C["ada_w"][layer, :, blk * 512:(blk + 1) * 512].rearrange("(kc k) n -> k kc n", k=128), writes=[dw])
            p, dp = pr.next()
            for kc in range(8):
                S.mm(p[:], CB[:, kc, :], w[:, kc, :], kc == 0, kc == 7, reads=[dw, dCB], writes=[dp])
            o, do = orr.next()
            ch, half = blk // 2, blk % 2
            S.tt("dve", o[:], p[:], bb[:, blk * 512:(blk + 1) * 512], ALU.add, reads=[dp, dbb], writes=[do])
            if ch in (1, 4):
                S.stt(o[:], o[:], 1.0, gb[:, 0 if ch == 1 else 1, half * 512:(half + 1) * 512], ALU.add, ALU.mult, reads=[dgb], writes=[do])
            S.dma("sp", so, C["modb"][layer, row_of[ch]:row_of[ch] + 1, half * 512:(half + 1) * 512], o[0:1, :], reads=[do], writes=[dout])
        S.finish()


def load_mod_rows(S, C, layer, rows, name):
    t = S.sb([128, len(rows), 1024], F32, name)
    d = Dep(name)
    st = S.dma_stream(name)
    for j, r in enumerate(rows):
        S.dma("sp", st, t[:, j, :], bcast_row(C["modb"][layer, r:r + 1, :], 1024), writes=[d])
    return t, d


class NormT:
    def __init__(self, S, K, AB, dAB, npT=2):
        self.S, self.K, self.AB, self.dAB = S, K, AB, dAB
        self.st = Ring(S, 2, [128, 4], F32, "nst")
        self.h32 = Ring(S, 2, [128, 1024], F32, "nh32")
        self.hb = Ring(S, 2, [128, 1024], BF16, "nhb")
        self.pT = Ring(S, npT, [128, 8, 128], BF16, "npT", psum=True)

    def run(self, x, dx, hT_dst, dhT, keep32=False, transpose=True):
        S, K = self.S, self.K
        st, dst = self.st.next()
        h32, dh32 = self.h32.next()
        S.act(h32[:], x, AF.Square, accum_out=st[:, 0:1], reads=[dx], writes=[dh32, dst])
        S.ts("dve", st[:, 1:2], st[:, 0:1], 1.0 / D, EPS, ALU.mult, ALU.add, reads=[dst], writes=[dst])
        S.op("act", lambda e: e.sqrt(st[:, 1:2], st[:, 1:2]), reads=[dst], writes=[dst])
        S.op("dve", lambda e: e.reciprocal(st[:, 2:3], st[:, 1:2]), reads=[dst], writes=[dst])
        S.stt(h32[:], x, st[:, 2:3], self.AB[:, 0, :], ALU.mult, ALU.mult, reads=[dx, dst, self.dAB], writes=[dh32])
        hb, dhb = self.hb.next()
        if keep32:
            S.tt("dve", h32[:], h32[:], self.AB[:, 1, :], ALU.add, reads=[self.dAB], writes=[dh32])
            S.cp("pool", hb[:], h32[:], reads=[dh32], writes=[dhb])
        else:
            S.tt("pool", hb[:], h32[:], self.AB[:, 1, :], ALU.add, reads=[dh32, self.dAB], writes=[dhb])
        if not transpose:
            return h32, dh32, hb, dhb
        pT, dpT = self.pT.next()
        for c in range(8):
            S.tr(pT[:, c, :], hb[:, c * 128:(c + 1) * 128], K["idb"][:], reads=[dhb, K["dep"]], writes=[dpT])
        S.cp("act", hT_dst, pT[:], reads=[dpT], writes=[dhT])
        return h32, dh32


def phase_l0mix(nc, C, NCH, name):
    with ExitStack() as es:
        S = Sch(nc, es, name)
        K = load_consts(S, C)
        st = S.dma_stream("ld")
        win = S.sb([128, 8, 2560], BF16, "win")
        dwin = Dep()
        wst = S.dma_stream("w")
        for kc in range(8):
            S.dma("pool", wst, win[:, kc, :], C["ab_w_in"][0, kc * 128:(kc + 1) * 128, :], writes=[dwin])
        wout = S.sb([128, 8, 1024], BF16, "wout")
        dwout = Dep()
        for kc in range(8):
            S.dma("pool", wst, wout[:, kc, :], C["ab_w_out"][0, kc * 128:(kc + 1) * 128, :], writes=[dwout])
        ca = S.sb([128, 4, 3], F32, "ca")
        cb = S.sb([128, 4, 31], F32, "cb")
        pp = S.sb([128, 4, 3], F32, "pp")
        hfl = S.sb([128, NCH], F32, "hfl")
        dpar = Dep()
        S.dma("sp", st, ca[:], C["ca_arr"], writes=[dpar])
        S.dma("sp", st, cb[:], C["cb_arr"], writes=[dpar])
        S.dma("sp", st, pp[:], C["pp_arr"], writes=[dpar])
        S.dma("sp", st, hfl[:], bcast_row(C["hflag"], NCH), writes=[dpar])
        AB, dAB = load_mod_rows(S, C, 0, [0, 1, 2], "AB")
        dg = S.sb([128, 4, 31, 128], BF16, "dg")
        ddg = Dep()
        for cc in range(4):
            for k in range(31):
                S.ts("pool" if (k % 2) else "dve", dg[:, cc, k, :], K["idb"][:], cb[:, cc, k:k + 1], None, ALU.mult,
                     reads=[K["dep"], dpar], writes=[ddg])
        NT = NormT(S, K, AB, dAB)
        xr = Ring(S, 1, [128, 5, 1024], F32, "x")
        xst = [S.dma_stream("x0"), S.dma_stream("x1")]
        hTr = Ring(S, 1, [128, 8, 640], BF16, "hT")
        pz = Ring(S, 6, [128, 512], F32, "pz", psum=True)
        xa_sb = Ring(S, 2, [128, 640], F32, "xa")
        v_sb = Ring(S, 2, [128, 640], F32, "v")
        acc_sb = Ring(S, 2, [128, 512], F32, "acc")
        sg_sb = Ring(S, 2, [128, 640], F32, "sg")
        u_sb = Ring(S, 2, [128, 640], BF16, "u")
        uc = S.sb([128, 4, 512], F32, "uc")
        duc = [Dep() for _ in range(4)]
        sq = S.sb([128, 4, 512], F32, "sq")
        dsq = [Dep() for _ in range(4)]
        ycat_r = Ring(S, 1, [128, 8, 512], BF16, "ycat")
        mean = S.sb([128, 512], F32, "mean")
        rstd = S.sb([128, 512], F32, "rstd")
        dmr = Dep()
        t1r = Ring(S, 2, [128, 512], F32, "t1")
        x1r = Ring(S, 2, [128, 1024], F32, "x1")
        sto = S.dma_stream("sto")
        dX1 = Dep()
        for i in range(NCH):
            x, dx = xr.next()
            S.dma("sp", xst[i % 2], x[:, 0, :], C["xh"][i], writes=[dx])
            S.dma("sp", xst[i % 2], x[:, 1:5, :], C["xc"][i * 512:(i + 1) * 512, :].rearrange("(t p) d -> p t d", p=128), writes=[dx])
            hT, dhT = hTr.next()
            for t in range(5):
                NT.run(x[:, t, :], dx, hT[:, :, t * 128:(t + 1) * 128], dhT)
            ycat, dycat = ycat_r.next()

            def proj(colchunk, halo):
                res = []
                pm, dpm = pz.next()
                for kc in range(8):
                    S.mm(pm[:], win[:, kc, colchunk * 128:(colchunk + 1) * 128], hT[:, kc, 128:640], kc == 0, kc == 7,
                         reads=[dwin, dhT], writes=[dpm])
                res.append((pm, dpm))
                if halo:
                    ph, dph = pz.next()
                    for kc in range(8):
                        S.mm(ph[:, 0:128], win[:, kc, colchunk * 128:(colchunk + 1) * 128], hT[:, kc, 0:128], kc == 0, kc == 7,
                             reads=[dwin, dhT], writes=[dph])
                    res.append((ph, dph))
                return res
            for cc in range(4):
                (pgc, dgc), (pgch, dgch) = proj(4 + cc, True)
                (pxa, dxa), (pxah, dxah) = proj(8 + cc, True)
                xa, dxs = xa_sb.next()
                S.cp("act", xa[:, 128:640], pxa[:], reads=[dxa], writes=[dxs])
                S.cp("act", xa[:, 0:128], pxah[:, 0:128], reads=[dxah], writes=[dxs])
                v, dv = v_sb.next()
                S.tt("dve", v[:, 128:640], pgc[:], xa[:, 128:640], ALU.mult, reads=[dgc, dxs], writes=[dv])
                S.stt(v[:, 0:128], pgch[:, 0:128], hfl[:, i:i + 1], xa[:, 0:128], ALU.mult, ALU.mult, reads=[dgch, dxs, dpar], writes=[dv])
                acc, dacc = acc_sb.next()
                S.ts("dve", acc[:], v[:, 126:638], ca[:, cc, 0:1], None, ALU.mult, reads=[dv, dpar], writes=[dacc])
                S.stt(acc[:], v[:, 127:639], ca[:, cc, 1:2], acc[:], ALU.mult, ALU.add, reads=[dv], writes=[dacc])
                S.stt(acc[:], v[:, 128:640], ca[:, cc, 2:3], acc[:], ALU.mult, ALU.add, reads=[dv], writes=[dacc])
                ((pgb, dgb),) = proj(cc, False)
                S.tt("dve", ycat[:, cc, :], pgb[:], acc[:], ALU.mult, reads=[dgb, dacc], writes=[dycat])
                (pvb, dvb), (pvbh, dvbh) = proj(12 + cc, True)
                (pgg, dgg), (pggh, dggh) = proj(16 + cc, True)
                sg, dsg = sg_sb.next()
                S.act(sg[:, 128:640], pgg[:], AF.Sigmoid, reads=[dgg], writes=[dsg])
                S.act(sg[:, 0:128], pggh[:, 0:128], AF.Sigmoid, reads=[dggh], writes=[dsg])
                u, du = u_sb.next()
                S.tt("dve", u[:, 128:640], pvb[:], sg[:, 128:640], ALU.mult, reads=[dvb, dsg], writes=[du])
                S.stt(u[:, 0:128], pvbh[:, 0:128], hfl[:, i:i + 1], sg[:, 0:128], ALU.mult, ALU.mult, reads=[dvbh, dsg, dpar], writes=[du])
                pcv, dcv = pz.next()
                for k in range(31):
                    S.mm(pcv[:], dg[:, cc, k, :], u[:, 98 + k:98 + k + 512], k == 0, k == 30, reads=[ddg, du], writes=[dcv])
                S.act(uc[:, cc, :], pcv[:], AF.Identity, bias=pp[:, cc, 0:1], reads=[dcv, dpar], writes=[duc[cc]])
                S.tt("pool", sq[:, cc, :], uc[:, cc, :], uc[:, cc, :], ALU.mult, reads=[duc[cc]], writes=[dsq[cc]])
            ps1, dps1 = pz.next()
            for cc in range(4):
                S.mm(ps1[:], K["onesf"][:], uc[:, cc, :], cc == 0, cc == 3, reads=[duc[cc], K["dep"]], writes=[dps1])
            ps2, dps2 = pz.next()
            for cc in range(4):
                S.mm(ps2[:], K["onesf"][:], sq[:, cc, :], cc == 0, cc == 3, reads=[dsq[cc], K["dep"]], writes=[dps2])
            S.ts("dve", mean[:], ps1[:], 1.0 / 512, None, ALU.mult, reads=[dps1], writes=[dmr])
            t1, dt1 = t1r.next()
            S.tt("dve", t1[:], mean[:], mean[:], ALU.mult, reads=[dmr], writes=[dt1])
            S.stt(rstd[:], ps2[:], 1.0 / 512, t1[:], ALU.mult, ALU.subtract, reads=[dps2, dt1], writes=[dmr])
            S.ts("dve", rstd[:], rstd[:], EPS, None, ALU.add, reads=[dmr], writes=[dmr])
            S.op("act", lambda e: e.sqrt(rstd[:], rstd[:]), reads=[dmr], writes=[dmr])
            S.op("dve", lambda e: e.reciprocal(rstd[:], rstd[:]), reads=[dmr], writes=[dmr])
            for cc in range(4):
                t1, dt1 = t1r.next()
                S.tt("dve", t1[:], uc[:, cc, :], mean[:], ALU.subtract, reads=[duc[cc], dmr], writes=[dt1])
                S.tt("pool", t1[:], t1[:], rstd[:], ALU.mult, reads=[dmr], writes=[dt1])
                S.ts("dve", t1[:], t1[:], pp[:, cc, 1:2], pp[:, cc, 2:3], ALU.mult, ALU.add, reads=[dpar], writes=[dt1])
                S.act(ycat[:, 4 + cc, :], t1[:], AF.Silu, reads=[dt1], writes=[dycat])
            for ts_ in range(4):
                x1, dx1 = x1r.next()
                for half in range(2):
                    py, dpy = pz.next()
                    for c in range(8):
                        S.mm(py[:], ycat[:, c, ts_ * 128:(ts_ + 1) * 128], wout[:, c, half * 512:(half + 1) * 512], c == 0, c == 7,
                             reads=[dycat, dwout], writes=[dpy])
                    S.tt("dve", x1[:, half * 512:(half + 1) * 512], py[:], AB[:, 2, half * 512:(half + 1) * 512], ALU.mult,
                         reads=[dpy, dAB], writes=[dx1])
                S.tt("pool", x1[:], x1[:], x[:, 1 + ts_, :], ALU.add, reads=[dx], writes=[dx1])
                r0 = i * 512 + ts_ * 128
                S.dma("sp", sto, C["X1"][r0:r0 + 128, :], x1[:], reads=[dx1], writes=[dX1])
        S.finish()


def phase_moe(nc, C, layer, NCH, Xin, Xout, final, name):
    TOK = NCH * 512
    TB = min(2048, TOK)
    NTB = TOK // TB
    NT_ = TB // 128
    with ExitStack() as es:
        S = Sch(nc, es, name)
        K = load_consts(S, C)
        st = S.dma_stream("ld")
        AB, dAB = load_mod_rows(S, C, layer, [3, 4, 5], "AB")
        wr = S.sb([128, 8, 36], F32, "wr")
        brb = S.sb([128, 36], F32, "brb")
        dwr = Dep()
        S.dma("sp", st, wr[:, :, 0:4], C["moe_wr_g"][layer].rearrange("(kc k) n -> k kc n", k=128), writes=[dwr])
        S.dma("sp", st, wr[:, :, 4:36], C["moe_wr_e"][layer].rearrange("(kc k) n -> k kc n", k=128), writes=[dwr])
        S.dma("sp", st, brb[:, 0:4], bcast_row(C["moe_br_g"][layer:layer + 1, :], 4), writes=[dwr])
        S.dma("sp", st, brb[:, 4:36], bcast_row(C["moe_br_e"][layer:layer + 1, :], 32), writes=[dwr])
        if final:
            fng = S.sb([128, 1024], F32, "fng")
            dfng = Dep()
            S.dma("sp", st, fng[:], bcast_row(C["final_norm_g"], 1024), writes=[dfng])
        NT = NormT(S, K, AB, dAB, npT=1)
        xr = Ring(S, 2, [128, 1024], F32, "x")
        xst = [S.dma_stream("x0"), S.dma_stream("x1")]
        hT = S.sb([128, 8, TB], BF16, "hT")
        dhT = Dep()
        acc = S.sb([128, NT_, 1024], F32, "acc")
        dacc = [Dep() for _ in range(NT_)]
        gates = S.sb([128, NT_, 32], F32, "gates")
        dgat = [Dep() for _ in range(NT_)]
        p32 = Ring(S, 1, [128, 8, 128], F32, "p32", psum=True)
        hT32 = Ring(S, 1, [128, 8, 128], F32, "hT32")
        pl = Ring(S, 1, [128, 512], F32, "pl", psum=True)
        rt = Ring(S, 2, [128, 96], F32, "rt")
        wg_r = Ring(S, 2, [128, 8, 512], BF16, "wg")
        wu_r = Ring(S, 2, [128, 8, 512], BF16, "wu")
        wd_r = Ring(S, 2, [128, 4, 1024], BF16, "wd")
        wsts = [S.dma_stream("wa"), S.dma_stream("wb")]
        pgu = Ring(S, 2, [128, 512], F32, "pgu", psum=True)
        py_r = Ring(S, 2, [128, 512], F32, "py", psum=True)
        sl_r = Ring(S, 2, [128, 512], F32, "sl")
        aT_r = Ring(S, 2, [128, 4, 512], BF16, "aT")
        xo_r = Ring(S, 1, [128, 1024], F32, "xo")
        sto = S.dma_stream("sto")
        dXo = Dep()
        for tb in range(NTB):
            for t in range(NT_):
                x, dx = xr.next()
                r0 = tb * TB + t * 128
                S.dma("sp", xst[t % 2], x[:], Xin[r0:r0 + 128, :], writes=[dx])
                h32, dh32 = NT.run(x[:], dx, hT[:, :, t * 128:(t + 1) * 128], dhT, keep32=True)
                pp_, dpp = p32.next()
                for c in range(8):
                    S.tr(pp_[:, c, :], h32[:, c * 128:(c + 1) * 128], K["idf"][:], reads=[dh32, K["dep"]], writes=[dpp])
                h3, dh3 = hT32.next()
                S.cp("act", h3[:], pp_[:], reads=[dpp], writes=[dh3])
                plg, dplg = pl.next()
                for c in range(8):
                    S.mm(plg[:, 0:36], h3[:, c, :], wr[:, c, :], c == 0, c == 7, reads=[dh3, dwr], writes=[dplg])
                r, dr = rt.next()
                w = [dr]
                S.tt("dve", r[:, 0:36], plg[:, 0:36], brb[:], ALU.add, reads=[dplg, dwr], writes=w)
                S.op("dve", lambda e, r=r: e.reduce_max(r[:, 36:37], r[:, 0:4], axis=AX.X), reads=w, writes=w)
                S.ts("dve", r[:, 57:58], r[:, 36:37], -1.0, None, ALU.mult, reads=w, writes=w)
                S.act(r[:, 92:96], r[:, 0:4], AF.Exp, bias=r[:, 57:58], accum_out=r[:, 37:38], reads=w, writes=w)
                S.op("dve", lambda e, r=r: e.reciprocal(r[:, 38:39], r[:, 37:38]), reads=w, writes=w)
                S.ts("dve", r[:, 40:44], r[:, 0:4], r[:, 36:37], None, ALU.is_equal, reads=w, writes=w)
                S.ts("dve", r[:, 44:52], r[:, 4:12], r[:, 40:41], None, ALU.mult, reads=w, writes=w)
                for g in range(1, 4):
                    S.stt(r[:, 44:52], r[:, 4 + 8 * g:12 + 8 * g], r[:, 40 + g:41 + g], r[:, 44:52], ALU.mult, ALU.add, reads=w, writes=w)
                S.op("dve", lambda e, r=r: e.reduce_max(r[:, 52:53], r[:, 44:52], axis=AX.X), reads=w, writes=w)
                S.ts("dve", r[:, 60:68], r[:, 44:52], r[:, 52:53], None, ALU.is_equal, reads=w, writes=w)
                S.stt(r[:, 68:76], r[:, 60:68], -1e30, r[:, 44:52], ALU.mult, ALU.add, reads=w, writes=w)
                S.op("dve", lambda e, r=r: e.reduce_max(r[:, 53:54], r[:, 68:76], axis=AX.X), reads=w, writes=w)
                S.ts("dve", r[:, 76:84], r[:, 68:76], r[:, 53:54], None, ALU.is_equal, reads=w, writes=w)
                S.tt("dve", r[:, 54:55], r[:, 53:54], r[:, 52:53], ALU.subtract, reads=w, writes=w)
                S.act(r[:, 54:55], r[:, 54:55], AF.Exp, reads=w, writes=w)
                S.ts("dve", r[:, 55:56], r[:, 54:55], 1.0, None, ALU.add, reads=w, writes=w)
                S.op("dve", lambda e, r=r: e.reciprocal(r[:, 55:56], r[:, 55:56]), reads=w, writes=w)
                S.tt("dve", r[:, 56:57], r[:, 54:55], r[:, 55:56], ALU.mult, reads=w, writes=w)
                S.tt("dve", r[:, 55:56], r[:, 55:56], r[:, 38:39], ALU.mult, reads=w, writes=w)
                S.tt("dve", r[:, 56:57], r[:, 56:57], r[:, 38:39], ALU.mult, reads=w, writes=w)
                S.ts("dve", r[:, 84:92], r[:, 60:68], r[:, 55:56], None, ALU.mult, reads=w, writes=w)
                S.stt(r[:, 84:92], r[:, 76:84], r[:, 56:57], r[:, 84:92], ALU.mult, ALU.add, reads=w, writes=w)
                for g in range(4):
                    S.ts("dve", gates[:, t, 8 * g:8 * g + 8], r[:, 84:92], r[:, 40 + g:41 + g], None, ALU.mult, reads=w, writes=[dgat[t]])
            for e_ in range(32):
                wg, dwg = wg_r.next()
                wu, dwu = wu_r.next()
                wd, dwd = wd_r.next()
                ws_ = wsts[e_ % 2]
                S.dma("pool", ws_, wg[:], C["moe_w_gate"][layer, e_].rearrange("(kc k) n -> k kc n", k=128), writes=[dwg])
                S.dma("pool", ws_, wu[:], C["moe_w_up"][layer, e_].rearrange("(kc k) n -> k kc n", k=128), writes=[dwu])
                S.dma("pool", ws_, wd[:], C["moe_w_down"][layer, e_].rearrange("(kc k) n -> k kc n", k=128), writes=[dwd])
                for ch in range(TB // 512):
                    aT, daT = aT_r.next()
                    for f in range(4):
                        pg, dpg = pgu.next()
                        for kc in range(8):
                            S.mm(pg[:], wg[:, kc, f * 128:(f + 1) * 128], hT[:, kc, ch * 512:(ch + 1) * 512], kc == 0, kc == 7, reads=[dwg, dhT], writes=[dpg])
                        pu, dpu = pgu.next()
                        for kc in range(8):
                            S.mm(pu[:], wu[:, kc, f * 128:(f + 1) * 128], hT[:, kc, ch * 512:(ch + 1) * 512], kc == 0, kc == 7, reads=[dwu, dhT], writes=[dpu])
                        sl, dsl = sl_r.next()
                        S.act(sl[:], pg[:], AF.Silu, reads=[dpg], writes=[dsl])
                        S.tt("dve", aT[:, f, :], pu[:], sl[:], ALU.mult, reads=[dpu, dsl], writes=[daT])
                    for ts_ in range(4):
                        t = ch * 4 + ts_
                        for half in range(2):
                            py, dpy = py_r.next()
                            for f in range(4):
                                S.mm(py[:], aT[:, f, ts_ * 128:(ts_ + 1) * 128], wd[:, f, half * 512:(half + 1) * 512], f == 0, f == 3, reads=[daT, dwd], writes=[dpy])
                            dst_ = acc[:, t, half * 512:(half + 1) * 512]
                            if e_ == 0:
                                S.ts("dve", dst_, py[:], gates[:, t, e_:e_ + 1], None, ALU.mult, reads=[dpy, dgat[t]], writes=[dacc[t]])
                            else:
                                S.stt(dst_, py[:], gates[:, t, e_:e_ + 1], dst_, ALU.mult, ALU.add, reads=[dpy, dgat[t]], writes=[dacc[t]])
            for t in range(NT_):
                x, dx = xr.next()
                r0 = tb * TB + t * 128
                S.dma("sp", xst[t % 2], x[:], Xin[r0:r0 + 128, :], writes=[dx])
                xo, dxo = xo_r.next()
                S.tt("dve", xo[:], acc[:, t, :], AB[:, 2, :], ALU.mult, reads=[dacc[t], dAB], writes=[dxo])
                S.tt("pool", xo[:], xo[:], x[:], ALU.add, reads=[dx], writes=[dxo])
                if final:
                    r, dr = rt.next()
                    S.act(x[:], xo[:], AF.Square, accum_out=r[:, 0:1], reads=[dxo], writes=[dx, dr])
                    S.ts("dve", r[:, 1:2], r[:, 0:1], 1.0 / D, EPS, ALU.mult, ALU.add, reads=[dr], writes=[dr])
                    S.op("act", lambda e, r=r: e.sqrt(r[:, 1:2], r[:, 1:2]), reads=[dr], writes=[dr])
                    S.op("dve", lambda e, r=r: e.reciprocal(r[:, 2:3], r[:, 1:2]), reads=[dr], writes=[dr])
                    S.stt(xo[:], xo[:], r[:, 2:3], fng[:], ALU.mult, ALU.mult, reads=[dr, dfng], writes=[dxo])
                S.dma("sp", sto, Xout[r0:r0 + 128, :], xo[:], reads=[dxo], writes=[dXo])
        S.finish()


CD = dict(qc=0, kc=512, vc=1024, qi=1536, ki=1792, wi=1824, qd=1832, kd=2344, vd=2856)


def phase_l1proj(nc, C, NCH, Xin, name, NOWN=None):
    NOWN = NCH if NOWN is None else NOWN
    with ExitStack() as es:
        S = Sch(nc, es, name)
        K = load_consts(S, C)
        AB, dAB = load_mod_rows(S, C, 1, [0, 1], "AB")
        win = S.sb([128, 8, 3368], BF16, "win")
        dwin = Dep()
        wst = S.dma_stream("w")
        for kc in range(8):
            S.dma("pool", wst, win[:, kc, :], C["cd_w_in"][0, kc * 128:(kc + 1) * 128, :], writes=[dwin])
        NT = NormT(S, K, AB, dAB, npT=1)
        xr = Ring(S, 2, [128, 1024], F32, "x")
        xst = [S.dma_stream("x0"), S.dma_stream("x1")]
        hT = S.sb([128, 8, 512], BF16, "hT")
        dhT = Dep()
        pf = Ring(S, 3, [128, 512], F32, "pf", psum=True)
        ptk = Ring(S, 3, [128, 512], F32, "ptk", psum=True)
        stg = {n: Ring(S, 1, [m, cnt, 512], BF16, "sg" + n) for n, m, cnt in (("qc", 64, 8), ("kc", 64, 8), ("qi", 32, 8), ("ki", 32, 1), ("qd", 64, 8), ("kd", 64, 8))}
        vstage = S.sb([128, 8, 4, 65], BF16, "vst")
        dvst = Dep()
        S.memset("pool", vstage[:], 1.0, writes=[dvst])
        vdst = S.sb([128, 4, 4, 128], BF16, "vdst")
        dvdst = Dep()
        wist = S.sb([128, 4, 8], F32, "wist")
        dwist = Dep()
        sto = S.dma_stream("sto")
        dO = Dep()
        groups = (("qc", 64, 8, 0.125, "QcT"), ("kc", 64, 8, None, "KcT"), ("qi", 32, 8, None, "QiT"), ("ki", 32, 1, None, "KiT"),
                  ("qd", 64, 8, 0.125, "QdT"), ("kd", 64, 8, None, "KdT"))
        tog = 0
        for i in range(NCH):
            for t in range(4):
                x, dx = xr.next()
                S.dma("sp", xst[t % 2], x[:], Xin[i * 512 + t * 128:i * 512 + (t + 1) * 128, :], writes=[dx])
                NT.run(x[:], dx, hT[:, :, t * 128:(t + 1) * 128], dhT)
            slot, il = i // NOWN, i % NOWN
            for gname, M, cnt, scl, dname in groups:
                if slot == 1 and gname[0] == "q":
                    continue
                sg, dsg = stg[gname].next()
                for hh in range(cnt):
                    p, dp = pf.next()
                    c0 = CD[gname] + hh * M
                    for kc in range(8):
                        S.mm(p[0:M, :], win[:, kc, c0:c0 + M], hT[:, kc, :], kc == 0, kc == 7, reads=[dwin, dhT], writes=[dp])
                    tog += 1
                    if scl is not None:
                        if tog % 2:
                            S.act(sg[:, hh, :], p[0:M, :], AF.Copy, scale=scl, reads=[dp], writes=[dsg])
                        else:
                            S.ts("dve", sg[:, hh, :], p[0:M, :], scl, None, ALU.mult, reads=[dp], writes=[dsg])
                    else:
                        S.cp("act" if tog % 2 else "dve", sg[:, hh, :], p[0:M, :], reads=[dp], writes=[dsg])
                dst_ = C[dname] if gname[0] == "q" else C[dname][slot]
                S.dma("sp", sto, dst_[:, :, il * 512:(il + 1) * 512].rearrange("h m t -> m h t"), sg[:], reads=[dsg], writes=[dO])
            for t in range(4):
                p, dp = ptk.next()
                for kc in range(8):
                    S.mm(p[:], hT[:, kc, t * 128:(t + 1) * 128], win[:, kc, CD["vc"]:CD["vc"] + 512], kc == 0, kc == 7, reads=[dwin, dhT], writes=[dp])
                S.cp("act", vstage[:, :, t, 0:64], p[:].rearrange("p (h e) -> p h e", h=8), reads=[dp], writes=[dvst])
                p, dp = ptk.next()
                for kc in range(8):
                    S.mm(p[:], hT[:, kc, t * 128:(t + 1) * 128], win[:, kc, CD["vd"]:CD["vd"] + 512], kc == 0, kc == 7, reads=[dwin, dhT], writes=[dp])
                S.cp("dve", vdst[:, :, t, :], p[:].rearrange("p (h e) -> p h e", h=4), reads=[dp], writes=[dvdst])
                p, dp = ptk.next()
                for kc in range(8):
                    S.mm(p[:, 0:8], hT[:, kc, t * 128:(t + 1) * 128], win[:, kc, CD["wi"]:CD["wi"] + 8], kc == 0, kc == 7, reads=[dwin, dhT], writes=[dp])
                S.cp("dve", wist[:, t, :], p[:, 0:8], reads=[dp], writes=[dwist])
            S.dma("sp", sto, C["Vc"][slot][:, :, 4 * il:4 * il + 4, :].rearrange("h p t e -> p h t e"), vstage[:], reads=[dvst], writes=[dO])
            S.dma("sp", sto, C["Vd"][slot][:, :, 4 * il:4 * il + 4, :].rearrange("h p t e -> p h t e"), vdst[:], reads=[dvdst], writes=[dO])
            if slot == 0:
                S.dma("sp", sto, C["Wi"][:, 4 * il:4 * il + 4, :], wist[:], reads=[dwist], writes=[dO])
        S.finish()


import math
LAM_INIT = 0.8 - 0.6 * math.exp(-0.3)
U8 = mybir.dt.uint8
NBIS = 16


def phase_attn(nc, C, NCH, name):
    TOK = NCH * 512
    with ExitStack() as es:
        S = Sch(nc, es, name)
        K = load_consts(S, C)
        st = S.dma_stream("ld")
        dK = Dep()
        cI = {}
        for n in ("I1b", "negIb", "negI0b"):
            cI[n] = S.sb([128, 128], BF16, n)
            S.dma("sp", st, cI[n][:], C[n], writes=[dK])
        ones4 = S.sb([128, 512], BF16, "ones4")
        S.memset("pool", ones4[:], 1.0, writes=[dK])
        onesb = ones4[:, 0:128]
        CM = S.sb([128, 4, 1024], BF16, "CM")
        S.dma("sp", st, CM[:], C["CM"].rearrange("j p k -> p j k"), writes=[dK])
        rbt = S.sb([33, 12], F32, "rbt")
        S.dma("sp", st, rbt[0:32, :], C["rel_bias"], writes=[dK])
        S.memset("pool", rbt[32:33, :], 1.0, writes=[dK])
        tab = S.sb([33, 2, 255], F32, "tab")
        S.dma("sp", st, tab[:], C["t5tab"], writes=[dK])
        b31 = S.sb([128, 12], F32, "b31")
        S.dma("sp", st, b31[:], bcast_row(C["rel_bias"][31:32, :], 12), writes=[dK])
        Tdf = S.sb([128, 2, 12, 128], BF16, "Tdf")
        dTdf = Dep()
        pT5 = Ring(S, 1, [128, 32, 12], F32, "pT5", psum=True)
        for ty in range(2):
            for qb in range(4):
                p, dp = pT5.next()
                for qq in range(32):
                    q = qb * 32 + qq
                    S.mm(p[:, qq, :], tab[:, ty, 127 - q:255 - q], rbt[:], True, True, reads=[dK], writes=[dp])
                S.cp("dve", Tdf[:, ty, :, qb * 32:(qb + 1) * 32], p[:].rearrange("p q h -> p h q"), reads=[dp], writes=[dTdf])
        dl = S.sb([128, 256], F32, "dl")
        S.dma("sp", st, dl[:], bcast_row(C["diff_lam"][0:1].rearrange("o a b -> o (a b)"), 256), writes=[dK])
        prod = S.sb([128, 2, 64], F32, "prod")
        lam4 = S.sb([128, 8], F32, "lam4")
        dlam = Dep()
        S.tt("dve", prod[:, 0, :], dl[:, 0:64], dl[:, 64:128], ALU.mult, reads=[dK], writes=[dlam])
        S.tt("dve", prod[:, 1, :], dl[:, 128:192], dl[:, 192:256], ALU.mult, reads=[dK], writes=[dlam])
        S.op("dve", lambda e: e.reduce_sum(lam4[:, 0:1], prod[:, 0, :], axis=AX.X), reads=[dlam], writes=[dlam])
        S.op("dve", lambda e: e.reduce_sum(lam4[:, 1:2], prod[:, 1, :], axis=AX.X), reads=[dlam], writes=[dlam])
        S.act(lam4[:, 2:4], lam4[:, 0:2], AF.Exp, reads=[dlam], writes=[dlam])
        S.tt("dve", lam4[:, 4:5], lam4[:, 2:3], lam4[:, 3:4], ALU.subtract, reads=[dlam], writes=[dlam])
        S.ts("dve", lam4[:, 5:6], lam4[:, 4:5], -1.0, -LAM_INIT, ALU.mult, ALU.add, reads=[dlam], writes=[dlam])
        dng = S.sb([128, 1], F32, "dng")
        S.dma("sp", st, dng[:], C["dng_col"], writes=[dlam])
        S.ts("dve", dng[:], dng[:], 1.0 - LAM_INIT, None, ALU.mult, reads=[dlam], writes=[dlam])
        kiT = S.sb([32, 2, TOK], BF16, "kiT")
        S.dma("sp", st, kiT[:], C["KiT2"].rearrange("r m t -> m r t"), writes=[dK])
        qiT = S.sb([32, 8, 512], BF16, "qiT")
        wi = S.sb([128, 4, 8], F32, "wi")
        dqi = Dep()
        qst = S.dma_stream("q")
        M01 = S.sb([128, 8 * NCH, 512], BF16, "M01")
        dM = Dep()
        score = S.sb([128, 1024 * NCH], F32, "score")
        dsc = Dep()
        junk = S.sb([128, 1024 * NCH], U8, "junk")
        dj = Dep()
        tmp_r = Ring(S, 4, [128, 512], BF16, "tmp")
        bs_r = Ring(S, 2, [128, 16], F32, "bs")
        sgnD = S.sb([128, 8, 128], BF16, "sgnD")
        wsg = S.sb([128, 2, 8], F32, "wsg")
        dsg = Dep()
        pow2 = S.sb([128, NBIS], F32, "pow2")
        for k_ in range(NBIS):
            S.memset("pool", pow2[:, k_:k_ + 1], 2.0 ** -(k_ + 1), writes=[dK])
        stp = S.sb([128, NBIS], F32, "stp")
        dgt = S.sb([128, 128], F32, "dgt")
        thrbc = S.sb([128, 128], F32, "thrbc")
        dthr = Dep()
        pS = Ring(S, 3, [128, 512], F32, "pS", psum=True)
        pO = S.ps([128, 512], F32, "pO")
        dpO = Dep()
        pD = S.ps([128, 512], F32, "pD")
        dpD = Dep()
        pB = S.ps([128, 512], F32, "pB")
        dpB = Dep()
        pA = S.ps([128, 512], F32, "pA")
        dpA = Dep()
        accs = [(pA, dpA), (pB, dpB)]
        kT = S.sb([64, 2, 512 * NCH], BF16, "kT")
        dkT = Dep()
        kst = S.dma_stream("k")
        vd = S.sb([128, 2, 4 * NCH, 128], BF16, "vd")
        dvd = Dep()
        vv = vd
        dvv = dvd
        vst = S.dma_stream("v")
        qT = S.sb([64, 512], BF16, "qT")
        dqT = Dep()
        pT_r = Ring(S, 3, [128, 512], BF16, "pT")
        pM_r = Ring(S, 3, [128, 512], BF16, "pM")
        rec = S.sb([128, 512], F32, "rec")
        drec = Dep()
        bc = S.sb([128, 512], F32, "bc")
        dbc = Dep()
        o1 = S.sb([128, 512], F32, "o1")
        o2 = S.sb([128, 512], F32, "o2")
        do12 = Dep()
        OcT_r = Ring(S, 2, [64, 512], BF16, "OcT")
        OdT_r = Ring(S, 2, [128, 512], BF16, "OdT")
        sto = S.dma_stream("sto")
        dOut = Dep()

        def ttype_ops(p, rk, s, hidx, need_mask):
            ops = []
            if rk == 0:
                for j in range(4):
                    dlt = j - s
                    sl = p[:, j * 128:(j + 1) * 128]
                    if dlt >= 2:
                        continue
                    if dlt == 1:
                        ops.append((sl, K["idb"], Tdf[:, 1, hidx, :]))
                    elif dlt == 0:
                        ops.append((sl, K["idb"], Tdf[:, 0, hidx, :]))
                    elif need_mask:
                        ops.append((sl, cI["negIb"], ones4[:, 0:128]))
            else:
                if need_mask:
                    ops.append((p[:], cI["negI0b"], ones4[:]))
                if s == 3:
                    ops.append((p[:, 0:128], cI["I1b"], Tdf[:, 1, hidx, :]))
            return ops

        def emit_ops(ops, dp):
            for n_, (sl, l, r_) in enumerate(ops):
                S.mm(sl, l[:], r_, False, n_ == len(ops) - 1, reads=[dK, dTdf, K["dep"]], writes=[dp])

        for i in range(NCH):
            nch = i + 1
            nkt = 8 * nch
            nk = nkt * 128
            tiles = [(c, rk, s) for c in range(nch) for rk in range(2) for s in range(4)]
            S.dma("sp", qst, qiT[:], C["QiT"][:, :, i * 512:(i + 1) * 512].rearrange("h m t -> m h t"), writes=[dqi])
            S.dma("sp", qst, wi[:], C["Wi"][:, 4 * i:4 * i + 4, :], writes=[dqi])
            for j in range(4):
                S.stt(wsg[:, 0, :], wi[:, j, :], -1.0, wi[:, j, :], ALU.mult, ALU.max, reads=[dqi], writes=[dsg])
                S.act(wsg[:, 1, :], wi[:, j, :], AF.Sign, reads=[dqi], writes=[dsg])
                for h in range(8):
                    S.ts("dve", sgnD[:, h, :], K["idb"][:], wsg[:, 1, h:h + 1], None, ALU.mult, reads=[dsg, K["dep"]], writes=[dsg])
                units = [(kc_, h) for kc_ in range(2 * nch) for h in range(8)]
                live = {}

                def stA(n):
                    kc_, h = units[n]
                    c, rk = kc_ // 2, kc_ % 2
                    p_, dp_ = pS.next()
                    S.mm(p_[:], qiT[:, h, j * 128:(j + 1) * 128], kiT[:, rk, c * 512:(c + 1) * 512], True, True, reads=[dqi, dK], writes=[dp_])
                    live[n] = (p_, dp_)

                def stBC(n):
                    kc_, h = units[n]
                    p_, dp_ = live.pop(n)
                    tmp, dtmp = tmp_r.next()
                    if h % 2 == 0:
                        S.ts("dve", tmp[:], p_[:], 0.0, wsg[:, 0, h:h + 1], ALU.max, ALU.mult, reads=[dp_, dsg], writes=[dtmp])
                    else:
                        S.act(tmp[:], p_[:], AF.Relu, scale=wsg[:, 0, h:h + 1], reads=[dp_, dsg], writes=[dtmp])
                    pa, dpa = accs[kc_ % 2]
                    S.mm(pa[:], sgnD[:, h, :], tmp[:], h == 0, h == 7, reads=[dtmp, dsg], writes=[dpa])
                    if h == 7:
                        S.cp("act" if kc_ % 2 else "dve", score[:, kc_ * 512:(kc_ + 1) * 512], pa[:], reads=[dpa], writes=[dsc])
                LOOK = 2
                for n in range(len(units) + LOOK):
                    if n < len(units):
                        stA(n)
                    if n - LOOK >= 0:
                        stBC(n - LOOK)
                bs, dbs = bs_r.next()
                wb = [dbs]
                S.op("dve", lambda e, bs=bs, nk=nk: e.reduce_max(bs[:, 0:1], score[:, 0:nk], axis=AX.X, apply_absolute_value=True), reads=[dsc], writes=wb)
                S.tt("pool", score[:, nk - 1024:nk], score[:, nk - 1024:nk], CM[:, j, :], ALU.add, reads=[dK, dbs], writes=[dsc])
                S.ts("dve", bs[:, 1:2], bs[:, 0:1], -1.001, -1e-3, ALU.mult, ALU.add, reads=wb, writes=wb)
                S.ts("dve", bs[:, 2:3], bs[:, 0:1], 2.002, 2e-3, ALU.mult, ALU.add, reads=wb, writes=wb)
                S.ts("dve", stp[:], pow2[:], bs[:, 2:3], None, ALU.mult, reads=wb + [dK], writes=wb)
                for it in range(NBIS):
                    S.tt("dve", bs[:, 3:4], bs[:, 1:2], stp[:, it:it + 1], ALU.add, reads=wb, writes=wb)
                    S.ts("dve", junk[:, 0:nk], score[:, 0:nk], bs[:, 3:4], None, ALU.is_gt, ALU.add, reads=[dsc, dbs], writes=[dj, dbs], accum_out=bs[:, 4:5])
                    S.stt(bs[:, 5:6], bs[:, 4:5], 255.5, stp[:, it:it + 1], ALU.is_gt, ALU.mult, reads=wb, writes=wb)
                    S.tt("dve", bs[:, 1:2], bs[:, 1:2], bs[:, 5:6], ALU.add, reads=wb, writes=wb)
                S.ts("dve", dgt[:], K["idf"][:], bs[:, 1:2], None, ALU.mult, reads=[dbs, K["dep"]], writes=[dthr])
                S.mm(pB[:, 0:128], K["onesf"][:], dgt[:], True, True, reads=[dthr, K["dep"]], writes=[dpB])
                S.cp("act", thrbc[:], pB[:, 0:128], reads=[dpB], writes=[dthr])
                for g4 in range(nkt // 4):
                    p, dp = pS.next()
                    for u in range(4):
                        kt = g4 * 4 + u
                        S.tr(p[:, u * 128:(u + 1) * 128], score[:, kt * 128:(kt + 1) * 128], K["idf"][:], reads=[dsc, K["dep"]], writes=[dp])
                    for u in range(4):
                        kt = g4 * 4 + u
                        S.tt("dve", M01[:, kt, j * 128:(j + 1) * 128], p[:, u * 128:(u + 1) * 128], thrbc[:], ALU.is_gt, reads=[dp, dthr], writes=[dM])
            for h in range(8):
                for rk in range(2):
                    S.dma("sp", kst, kT[:, rk, 0:nch * 512], C["KcT2"][rk, h, :, 0:nch * 512], writes=[dkT])
                    S.dma("sp", vst, vv[:, rk, 0:4 * nch, 0:65], C["Vc2"][rk, h, :, 0:4 * nch, :], writes=[dvv])
                S.dma("sp", kst, qT[:], C["QcT"][h, :, i * 512:(i + 1) * 512], writes=[dqT])
                live = {}

                def hA(n_, h=h):
                    c, rk, s = tiles[n_]
                    p, dp = pS.next()
                    ops = ttype_ops(p, rk, s, h, False) if c == i else []
                    S.mm(p[:], kT[:, rk, c * 512 + s * 128:c * 512 + (s + 1) * 128], qT[:], True, len(ops) == 0, reads=[dkT, dqT], writes=[dp])
                    emit_ops(ops, dp)
                    live[n_] = (p, dp)

                def hBC(n_, h=h):
                    c, rk, s = tiles[n_]
                    p, dp = live.pop(n_)
                    pT, dpT = pT_r.next()
                    S.act(pT[:], p[:], AF.Exp, bias=b31[:, h:h + 1], reads=[dp, dK], writes=[dpT])
                    pM, dpM = pM_r.next()
                    S.tt("dve", pM[:], pT[:], M01[:, n_, :], ALU.mult, reads=[dpT, dM], writes=[dpM])
                    S.mm(pO[0:65, :], vv[:, rk, c * 4 + s, 0:65], pM[:], n_ == 0, n_ == nkt - 1, reads=[dvv, dpM], writes=[dpO])
                for n_ in range(nkt + 2):
                    if n_ < nkt:
                        hA(n_)
                    if n_ - 2 >= 0:
                        hBC(n_ - 2)
                S.op("dve", lambda e: e.reciprocal(rec[64:65, :], pO[64:65, :]), reads=[dpO], writes=[drec])
                S.mm(pB[0:64, :], K["onesf"][64:65, 0:64], rec[64:65, :], True, True, reads=[drec, K["dep"]], writes=[dpB])
                S.cp("act", bc[0:64, :], pB[0:64, :], reads=[dpB], writes=[dbc])
                OcT, dOc = OcT_r.next()
                S.tt("dve", OcT[:], pO[0:64, :], bc[0:64, :], ALU.mult, reads=[dpO, dbc], writes=[dOc])
                S.dma("sp", sto, C["OcTd"][h, :, i * 512:(i + 1) * 512], OcT[:], reads=[dOc], writes=[dOut])
            for hd in range(4):
                for rk in range(2):
                    S.dma("sp", vst, vd[:, rk, 0:4 * nch, :], C["Vd2"][rk, hd, :, 0:4 * nch, :], writes=[dvd])
                for mp in range(2):
                    for rk in range(2):
                        S.dma("sp", kst, kT[:, rk, 0:nch * 512], C["KdT2"][rk, hd * 2 + mp, :, 0:nch * 512], writes=[dkT])
                    S.dma("sp", kst, qT[:], C["QdT"][hd * 2 + mp, :, i * 512:(i + 1) * 512], writes=[dqT])
                    live = {}

                    def dA(n_, hd=hd):
                        c, rk, s = tiles[n_]
                        p, dp = pS.next()
                        ops = ttype_ops(p, rk, s, 8 + hd, True) if c == i else []
                        S.mm(p[:], kT[:, rk, c * 512 + s * 128:c * 512 + (s + 1) * 128], qT[:], True, len(ops) == 0, reads=[dkT, dqT], writes=[dp])
                        emit_ops(ops, dp)
                        live[n_] = (p, dp)

                    def dBC(n_, hd=hd):
                        c, rk, s = tiles[n_]
                        p, dp = live.pop(n_)
                        pT, dpT = pT_r.next()
                        S.act(pT[:], p[:], AF.Exp, bias=b31[:, 8 + hd:9 + hd], reads=[dp, dK], writes=[dpT])
                        S.mm(pO[:], vd[:, rk, c * 4 + s, :], pT[:], n_ == 0, n_ == nkt - 1, reads=[dvd, dpT], writes=[dpO])
                        S.mm(pD[0:32, :], ones4[:, 0:32], pT[:], n_ == 0, n_ == nkt - 1, reads=[dK, dpT], writes=[dpD])
                    for n_ in range(nkt + 2):
                        if n_ < nkt:
                            dA(n_)
                        if n_ - 2 >= 0:
                            dBC(n_ - 2)
                    S.op("dve", lambda e: e.reciprocal(rec[0:1, :], pD[0:1, :]), reads=[dpD], writes=[drec])
                    S.mm(pB[:], K["onesf"][0:1, :], rec[0:1, :], True, True, reads=[drec, K["dep"]], writes=[dpB])
                    S.cp("act", bc[:], pB[:], reads=[dpB], writes=[dbc])
                    S.tt("dve", (o1 if mp == 0 else o2)[:], pO[:], bc[:], ALU.mult, reads=[dpO, dbc], writes=[do12])
                S.stt(o1[:], o2[:], lam4[:, 5:6], o1[:], ALU.mult, ALU.add, reads=[dlam, do12], writes=[do12])
                S.tt("pool", o2[:], o1[:], o1[:], ALU.mult, reads=[do12], writes=[do12])
                S.mm(pB[:], K["onesf"][:], o2[:], True, True, reads=[do12, K["dep"]], writes=[dpB])
                S.ts("dve", bc[:], pB[:], 1.0 / 128, EPS, ALU.mult, ALU.add, reads=[dpB], writes=[dbc])
                S.op("act", lambda e: e.sqrt(bc[:], bc[:]), reads=[dbc], writes=[dbc])
                S.op("dve", lambda e: e.reciprocal(bc[:], bc[:]), reads=[dbc], writes=[dbc])
                S.tt("dve", o1[:], o1[:], bc[:], ALU.mult, reads=[dbc, do12], writes=[do12])
                OdT, dOd = OdT_r.next()
                S.ts("dve", OdT[:], o1[:], dng[:, 0:1], None, ALU.mult, reads=[do12, dlam], writes=[dOd])
                S.dma("sp", sto, C["OdTd"][hd, :, i * 512:(i + 1) * 512], OdT[:], reads=[dOd], writes=[dOut])
        S.finish()


def phase_attn_out(nc, C, NCH, Xin, Xout, name):
    with ExitStack() as es:
        S = Sch(nc, es, name)
        G1, dG1 = load_mod_rows(S, C, 1, [2], "G1")
        wst = S.dma_stream("w")
        wo_c = S.sb([64, 8, 1024], BF16, "woc")
        wo_d = S.sb([128, 4, 1024], BF16, "wod")
        dwo = Dep()
        S.dma("pool", wst, wo_c[:], C["cd_w_out"][0, 0:512, :].rearrange("(h m) n -> m h n", m=64), writes=[dwo])
        S.dma("pool", wst, wo_d[:], C["cd_w_out"][0, 512:1024, :].rearrange("(h m) n -> m h n", m=128), writes=[dwo])
        oc_r = Ring(S, 2, [64, 8, 512], BF16, "oc")
        od_r = Ring(S, 2, [128, 4, 512], BF16, "od")
        ost = [S.dma_stream("o0"), S.dma_stream("o1")]
        xr = Ring(S, 2, [128, 1024], F32, "x")
        xst = [S.dma_stream("x0"), S.dma_stream("x1")]
        xo_r = Ring(S, 2, [128, 1024], F32, "xo")
        py_r = Ring(S, 4, [128, 512], F32, "py", psum=True)
        sto = S.dma_stream("sto")
        dO = Dep()
        for i in range(NCH):
            oc, doc = oc_r.next()
            od, dod = od_r.next()
            S.dma("sp", ost[i % 2], oc[:], C["OcTd"][:, :, i * 512:(i + 1) * 512].rearrange("h m t -> m h t"), writes=[doc])
            S.dma("sp", ost[i % 2], od[:], C["OdTd"][:, :, i * 512:(i + 1) * 512].rearrange("h m t -> m h t"), writes=[dod])
            for t in range(4):
                x, dx = xr.next()
                r0 = i * 512 + t * 128
                S.dma("sp", xst[t % 2], x[:], Xin[r0:r0 + 128, :], writes=[dx])
                xo, dxo = xo_r.next()
                for half in range(2):
                    py, dpy = py_r.next()
                    for h in range(8):
                        S.mm(py[:], oc[:, h, t * 128:(t + 1) * 128], wo_c[:, h, half * 512:(half + 1) * 512], h == 0, False, reads=[doc, dwo], writes=[dpy])
                    for h in range(4):
                        S.mm(py[:], od[:, h, t * 128:(t + 1) * 128], wo_d[:, h, half * 512:(half + 1) * 512], False, h == 3, reads=[dod, dwo], writes=[dpy])
                    S.tt("dve", xo[:, half * 512:(half + 1) * 512], py[:], G1[:, 0, half * 512:(half + 1) * 512], ALU.mult, reads=[dpy, dG1], writes=[dxo])
                S.tt("pool", xo[:], xo[:], x[:], ALU.add, reads=[dx], writes=[dxo])
                S.dma("sp", sto, Xout[r0:r0 + 128, :], xo[:], reads=[dxo], writes=[dO])
        S.finish()


def moe_cap_tiles(TOK):
    return max(1, -(-(3 * TOK // 16) // 128))


def phase_moe_sparse(nc, C, layer, NCH, Xin, Xout, final, name, ecap_key):
    TOK = NCH * 512
    NT_ = TOK // 128
    CT = moe_cap_tiles(TOK)
    CAP = CT * 128
    NS = 32 * CAP
    Xs = C["Xs"][0:NS + 128, :]
    Ys = C["Ys"][0:NS + 128, :]
    with ExitStack() as es:
        S = Sch(nc, es, name)
        K = load_consts(S, C)
        st = S.dma_stream("ld")
        AB, dAB = load_mod_rows(S, C, layer, [3, 4, 5], "AB")
        wr = S.sb([128, 8, 36], F32, "wr")
        brb = S.sb([128, 36], F32, "brb")
        Ls = S.sb([128, 128], F32, "Ls")
        ecap = S.sb([128, 32], F32, "ecap")
        dwr = Dep()
        S.dma("sp", st, wr[:, :, 0:4], C["moe_wr_g"][layer].rearrange("(kc k) n -> k kc n", k=128), writes=[dwr])
        S.dma("sp", st, wr[:, :, 4:36], C["moe_wr_e"][layer].rearrange("(kc k) n -> k kc n", k=128), writes=[dwr])
        S.dma("sp", st, brb[:, 0:4], bcast_row(C["moe_br_g"][layer:layer + 1, :], 4), writes=[dwr])
        S.dma("sp", st, brb[:, 4:36], bcast_row(C["moe_br_e"][layer:layer + 1, :], 32), writes=[dwr])
        S.dma("sp", st, Ls[:], C["Ls"], writes=[dwr])
        S.dma("sp", st, ecap[:], bcast_row(C[ecap_key], 32), writes=[dwr])
        if final:
            fng = S.sb([128, 1024], F32, "fng")
            dfng = Dep()
            S.dma("sp", st, fng[:], bcast_row(C["final_norm_g"], 1024), writes=[dfng])
        zt = S.sb([128, 4, 1024], BF16, "zt")
        dzt = Dep()
        S.memset("pool", zt[:], 0.0, writes=[dzt])
        dXs = Dep()
        dYs = Dep()
        zst = S.dma_stream("z")
        Xs_v = Xs.rearrange("(a p) d -> p a d", p=128)
        S.dma("sp", zst, Ys[NS:NS + 128, :], zt[:, 0, :], reads=[dzt], writes=[dYs])
        na = NS // 128
        for a0 in range(0, na, 4):
            n_ = min(4, na - a0)
            S.dma("sp", zst, Xs_v[:, a0:a0 + n_, :], zt[:, 0:n_, :], reads=[dzt], writes=[dXs])
        NT = NormT(S, K, AB, dAB, npT=1)
        xr = Ring(S, 2, [128, 1024], F32, "x")
        xst = [S.dma_stream("x0"), S.dma_stream("x1")]
        p32 = Ring(S, 1, [128, 8, 128], F32, "p32", psum=True)
        hT32 = Ring(S, 1, [128, 8, 128], F32, "hT32")
        plc = S.ps([128, 512], F32, "plc")
        dpl = Dep()
        dpc = Dep()
        rt = Ring(S, 2, [128, 96], F32, "rt")
        mt_r = Ring(S, 2, [128, 168], F32, "mt")
        msum = S.sb([128, 32], F32, "msum")
        dms = Dep()
        S.memset("dve", msum[:], 0.0, writes=[dms])
        slots_i = S.sb([128, NT_, 2], I32, "slots")
        gk = S.sb([128, NT_, 2], F32, "gk")
        dsl_ = [Dep() for _ in range(NT_)]
        sc_st = S.dma_stream("scat")
        settle = S.sb([128, 4], F32, "settle")
        S.memset("dve", settle[:], 1.0, writes=[dms])
        pend = []

        def scatter(t, hb, dhb):
            for k in range(2):
                S.dmaop("pool", sc_st, (lambda e, hb=hb, t=t, k=k: e.indirect_dma_start(
                    out=Xs, out_offset=bass.IndirectOffsetOnAxis(ap=slots_i[:, t, k:k + 1], axis=0), in_=hb[:], in_offset=None)),
                    reads=[dhb, dsl_[t]], writes=[dXs])
        for t in range(NT_):
            x, dx = xr.next()
            r0 = t * 128
            S.dma("sp", xst[t % 2], x[:], Xin[r0:r0 + 128, :], writes=[dx])
            h32, dh32, hb, dhb = NT.run(x[:], dx, None, None, keep32=True, transpose=False)
            pp_, dpp = p32.next()
            for c in range(8):
                S.tr(pp_[:, c, :], h32[:, c * 128:(c + 1) * 128], K["idf"][:], reads=[dh32, K["dep"]], writes=[dpp])
            h3, dh3 = hT32.next()
            S.cp("act", h3[:], pp_[:], reads=[dpp], writes=[dh3])
            for c in range(8):
                S.mm(plc[:, 0:36], h3[:, c, :], wr[:, c, :], c == 0, c == 7, reads=[dh3, dwr], writes=[dpl])
            r, dr = rt.next()
            w = [dr]
            S.tt("dve", r[:, 0:36], plc[:, 0:36], brb[:], ALU.add, reads=[dpl, dwr], writes=w)
            S.op("dve", lambda e, r=r: e.reduce_max(r[:, 36:37], r[:, 0:4], axis=AX.X), reads=w, writes=w)
            S.ts("dve", r[:, 57:58], r[:, 36:37], -1.0, None, ALU.mult, reads=w, writes=w)
            S.act(r[:, 92:96], r[:, 0:4], AF.Exp, bias=r[:, 57:58], accum_out=r[:, 37:38], reads=w, writes=w)
            S.op("dve", lambda e, r=r: e.reciprocal(r[:, 38:39], r[:, 37:38]), reads=w, writes=w)
            S.ts("dve", r[:, 40:44], r[:, 0:4], r[:, 36:37], None, ALU.is_equal, reads=w, writes=w)
            S.ts("dve", r[:, 44:52], r[:, 4:12], r[:, 40:41], None, ALU.mult, reads=w, writes=w)
            for g in range(1, 4):
                S.stt(r[:, 44:52], r[:, 4 + 8 * g:12 + 8 * g], r[:, 40 + g:41 + g], r[:, 44:52], ALU.mult, ALU.add, reads=w, writes=w)
            S.op("dve", lambda e, r=r: e.reduce_max(r[:, 52:53], r[:, 44:52], axis=AX.X), reads=w, writes=w)
            S.ts("dve", r[:, 60:68], r[:, 44:52], r[:, 52:53], None, ALU.is_equal, reads=w, writes=w)
            S.stt(r[:, 68:76], r[:, 60:68], -1e30, r[:, 44:52], ALU.mult, ALU.add, reads=w, writes=w)
            S.op("dve", lambda e, r=r: e.reduce_max(r[:, 53:54], r[:, 68:76], axis=AX.X), reads=w, writes=w)
            S.ts("dve", r[:, 76:84], r[:, 68:76], r[:, 53:54], None, ALU.is_equal, reads=w, writes=w)
            S.tt("dve", r[:, 54:55], r[:, 53:54], r[:, 52:53], ALU.subtract, reads=w, writes=w)
            S.act(r[:, 54:55], r[:, 54:55], AF.Exp, reads=w, writes=w)
            S.ts("dve", r[:, 55:56], r[:, 54:55], 1.0, None, ALU.add, reads=w, writes=w)
            S.op("dve", lambda e, r=r: e.reciprocal(r[:, 55:56], r[:, 55:56]), reads=w, writes=w)
            S.tt("dve", r[:, 56:57], r[:, 54:55], r[:, 55:56], ALU.mult, reads=w, writes=w)
            S.tt("dve", r[:, 55:56], r[:, 55:56], r[:, 38:39], ALU.mult, reads=w, writes=w)
            S.tt("dve", r[:, 56:57], r[:, 56:57], r[:, 38:39], ALU.mult, reads=w, writes=w)
            mt, dmt = mt_r.next()
            wm = [dmt]
            for g in range(4):
                S.ts("dve", mt[:, 8 * g:8 * g + 8], r[:, 60:68], r[:, 40 + g:41 + g], None, ALU.mult, reads=w + wm, writes=wm)
                S.ts("dve", mt[:, 32 + 8 * g:40 + 8 * g], r[:, 76:84], r[:, 40 + g:41 + g], None, ALU.mult, reads=w + wm, writes=wm)
            S.tt("dve", mt[:, 64:96], mt[:, 0:32], mt[:, 32:64], ALU.add, reads=wm, writes=wm)
            S.mm(plc[:, 64:96], Ls[:], mt[:, 64:96], True, False, reads=[dmt, dwr], writes=[dpc])
            S.mm(plc[:, 64:96], K["onesf"][:], msum[:], False, True, reads=[dms, K["dep"]], writes=[dpc])
            S.tt("dve", mt[:, 96:128], plc[:, 64:96], ecap[:], ALU.add, reads=[dpc, dwr] + wm, writes=wm)
            S.tt("pool", msum[:], msum[:], mt[:, 64:96], ALU.add, reads=[dmt], writes=[dms])
            for k in range(2):
                Mk = mt[:, 32 * k:32 * k + 32]
                S.tt("dve", mt[:, 128:160], Mk, mt[:, 96:128], ALU.mult, reads=wm, writes=wm)
                S.op("dve", lambda e, mt=mt, k=k: e.reduce_sum(mt[:, 160 + k:161 + k], mt[:, 128:160], axis=AX.X), reads=wm, writes=wm)
                S.tt("dve", mt[:, 128:160], Mk, plc[:, 64:96], ALU.mult, reads=wm + [dpc], writes=wm)
                S.op("dve", lambda e, mt=mt, k=k: e.reduce_sum(mt[:, 162 + k:163 + k], mt[:, 128:160], axis=AX.X), reads=wm, writes=wm)
                S.ts("dve", mt[:, 164 + k:165 + k], mt[:, 162 + k:163 + k], float(CAP) - 0.5, None, ALU.is_gt, reads=wm, writes=wm)
                S.ts("dve", mt[:, 166 + k:167 + k], mt[:, 164 + k:165 + k], -1.0, 1.0, ALU.mult, ALU.add, reads=wm, writes=wm)
                S.tt("dve", mt[:, 160 + k:161 + k], mt[:, 160 + k:161 + k], mt[:, 166 + k:167 + k], ALU.mult, reads=wm, writes=wm)
                S.stt(mt[:, 160 + k:161 + k], mt[:, 164 + k:165 + k], float(NS), mt[:, 160 + k:161 + k], ALU.mult, ALU.add, reads=wm, writes=wm)
                S.tt("dve", gk[:, t, k:k + 1], r[:, 55 + k:56 + k], mt[:, 166 + k:167 + k], ALU.mult, reads=wm + w, writes=[dsl_[t]])
            S.cp("dve", slots_i[:, t, :], mt[:, 160:162], reads=wm, writes=[dsl_[t]])
            pend.append((t, hb, dhb))
            if len(pend) > 1:
                scatter(*pend.pop(0))
        for _ in range(6):
            S.ts("dve", settle[:], settle[:], 1.0, None, ALU.mult, reads=[dsl_[NT_ - 1]], writes=[dsl_[NT_ - 1]])
        scatter(*pend.pop(0))
        wg_r = Ring(S, 2, [128, 8, 512], BF16, "wg")
        wu_r = Ring(S, 2, [128, 8, 512], BF16, "wu")
        wd_r = Ring(S, 2, [128, 4, 1024], BF16, "wd")
        wsts = [S.dma_stream("wa"), S.dma_stream("wb")]
        pgu = Ring(S, 2, [128, 512], F32, "pgu", psum=True)
        py_r = Ring(S, 2, [128, 512], F32, "py", psum=True)
        sl_r = Ring(S, 2, [128, 512], F32, "sl")
        aT_r = Ring(S, 2, [128, 4, 512], BF16, "aT")
        xs_r = Ring(S, 3, [128, 1024], BF16, "xs")
        xs_st = [S.dma_stream("xs0"), S.dma_stream("xs1"), S.dma_stream("xs2")]
        XT_r = Ring(S, 2, [128, 8, 512], BF16, "XT")
        ys_r = Ring(S, 2, [128, 1024], BF16, "ys")
        ysto = S.dma_stream("ysto")
        nx = 0
        for e_ in range(32):
            wg, dwg = wg_r.next()
            wu, dwu = wu_r.next()
            wd, dwd = wd_r.next()
            ws_ = wsts[e_ % 2]
            S.dma("pool", ws_, wg[:], C["moe_w_gate"][layer, e_].rearrange("(kc k) n -> k kc n", k=128), writes=[dwg])
            S.dma("pool", ws_, wu[:], C["moe_w_up"][layer, e_].rearrange("(kc k) n -> k kc n", k=128), writes=[dwu])
            S.dma("pool", ws_, wd[:], C["moe_w_down"][layer, e_].rearrange("(kc k) n -> k kc n", k=128), writes=[dwd])
            for g0 in range(0, CT, 4):
                n_ = min(4, CT - g0)
                N = n_ * 128
                XT, dXT = XT_r.next()
                for u in range(n_):
                    xs, dxs = xs_r.next()
                    row0 = (e_ * CT + g0 + u) * 128
                    S.dma("sp", xs_st[nx % 3], xs[:], Xs[row0:row0 + 128, :], reads=[dXs], writes=[dxs])
                    nx += 1
                    pT, dpT = NT.pT.next()
                    for c in range(8):
                        S.tr(pT[:, c, :], xs[:, c * 128:(c + 1) * 128], K["idb"][:], reads=[dxs, K["dep"]], writes=[dpT])
                    S.cp("act" if u % 2 else "dve", XT[:, :, u * 128:(u + 1) * 128], pT[:], reads=[dpT], writes=[dXT])
                aT, daT = aT_r.next()
                for f in range(4):
                    pg, dpg = pgu.next()
                    for kc in range(8):
                        S.mm(pg[:, 0:N], wg[:, kc, f * 128:(f + 1) * 128], XT[:, kc, 0:N], kc == 0, kc == 7, reads=[dwg, dXT], writes=[dpg])
                    pu, dpu = pgu.next()
                    for kc in range(8):
                        S.mm(pu[:, 0:N], wu[:, kc, f * 128:(f + 1) * 128], XT[:, kc, 0:N], kc == 0, kc == 7, reads=[dwu, dXT], writes=[dpu])
                    sl, dsl = sl_r.next()
                    S.act(sl[:, 0:N], pg[:, 0:N], AF.Silu, reads=[dpg], writes=[dsl])
                    S.tt("dve", aT[:, f, 0:N], pu[:, 0:N], sl[:, 0:N], ALU.mult, reads=[dpu, dsl], writes=[daT])
                for u in range(n_):
                    ys, dys = ys_r.next()
                    for half in range(2):
                        py, dpy = py_r.next()
                        for f in range(4):
                            S.mm(py[:], aT[:, f, u * 128:(u + 1) * 128], wd[:, f, half * 512:(half + 1) * 512], f == 0, f == 3, reads=[daT, dwd], writes=[dpy])
                        S.cp("act" if half else "dve", ys[:, half * 512:(half + 1) * 512], py[:], reads=[dpy], writes=[dys])
                    row0 = (e_ * CT + g0 + u) * 128
                    S.dma("sp", ysto, Ys[row0:row0 + 128, :], ys[:], reads=[dys], writes=[dYs])
        y_r = Ring(S, 2, [128, 2, 1024], BF16, "yg")
        for yt, _ in zip(y_r.t, range(2)):
            S.memset("pool", yt[:], 0.0, writes=[y_r.d[_]])
        g_sts = [S.dma_stream("gath0"), S.dma_stream("gath1")]
        xo_r = Ring(S, 2, [128, 1024], F32, "xo")
        sto = S.dma_stream("sto")
        dXo = Dep()
        for t in range(NT_):
            x, dx = xr.next()
            r0 = t * 128
            S.dma("sp", xst[t % 2], x[:], Xin[r0:r0 + 128, :], writes=[dx])
            yg, dyg = y_r.next()
            for k in range(2):
                S.dmaop("pool", g_sts[t % 2], (lambda e, yg=yg, t=t, k=k: e.indirect_dma_start(
                    out=yg[:, k, :], out_offset=None, in_=Ys, in_offset=bass.IndirectOffsetOnAxis(ap=slots_i[:, t, k:k + 1], axis=0))),
                    reads=[dYs, dsl_[t]], writes=[dyg])
            xo, dxo = xo_r.next()
            S.ts("dve", xo[:], yg[:, 0, :], gk[:, t, 0:1], None, ALU.mult, reads=[dyg, dsl_[t]], writes=[dxo])
            S.stt(xo[:], yg[:, 1, :], gk[:, t, 1:2], xo[:], ALU.mult, ALU.add, reads=[dyg, dsl_[t]], writes=[dxo])
            S.tt("dve", xo[:], xo[:], AB[:, 2, :], ALU.mult, reads=[dAB], writes=[dxo])
            S.tt("pool", xo[:], xo[:], x[:], ALU.add, reads=[dx], writes=[dxo])
            if final:
                r, dr = rt.next()
                S.act(x[:], xo[:], AF.Square, accum_out=r[:, 0:1], reads=[dxo], writes=[dx, dr])
                S.ts("dve", r[:, 1:2], r[:, 0:1], 1.0 / D, EPS, ALU.mult, ALU.add, reads=[dr], writes=[dr])
                S.op("act", lambda e, r=r: e.sqrt(r[:, 1:2], r[:, 1:2]), reads=[dr], writes=[dr])
                S.op("dve", lambda e, r=r: e.reciprocal(r[:, 2:3], r[:, 1:2]), reads=[dr], writes=[dr])
                S.stt(xo[:], xo[:], r[:, 2:3], fng[:], ALU.mult, ALU.mult, reads=[dr, dfng], writes=[dxo])
            S.dma("sp", sto, Xout[r0:r0 + 128, :], xo[:], reads=[dxo], writes=[dXo])
        S.finish()


import ml_dtypes
BF = ml_dtypes.bfloat16

def core_consts():
    return {"idb": np.eye(128).astype(BF), "idf": np.eye(128, dtype=np.float32)}

def prep_l0(inputs, b, r, NCH, dup=False):
    x = np.asarray(inputs["x"])
    d = {}
    chunks = [2 * i + r for i in range(NCH)]
    if dup:
        chunks = chunks + [2 * i + (1 - r) for i in range(NCH)]
    n = len(chunks)
    d["xc"] = np.ascontiguousarray(np.concatenate([x[b, 512 * g:512 * (g + 1)] for g in chunks], 0))
    xh = np.zeros((n, 128, 1024), np.float32)
    hf = np.ones((1, n), np.float32)
    for i, g in enumerate(chunks):
        if g == 0:
            hf[0, i] = 0.0
        else:
            xh[i] = x[b, 512 * g - 128:512 * g]
    d["xh"] = xh
    d["hflag"] = hf
    d["c_arr"] = np.ascontiguousarray(np.asarray(inputs["c"])[b].reshape(8, 128).T)
    def arr(v, n):
        return np.ascontiguousarray(np.asarray(v, np.float32).reshape(n, 4, 128).transpose(2, 1, 0))
    d["ca_arr"] = arr(inputs["ab_conv_a"][0], 3)
    d["cb_arr"] = arr(inputs["ab_conv_b"][0], 31)
    d["pp_arr"] = arr(np.stack([np.asarray(inputs["ab_conv_b_bias"][0]), np.asarray(inputs["ab_ln_g"][0]), np.asarray(inputs["ab_ln_b"][0])]), 3)
    return d

import math
def t5_tab():
    def bucket(n):
        n = np.maximum(n, 0)
        lr = np.log(np.maximum(n, 1).astype(np.float32) / np.float32(16)) / np.float32(math.log(8))
        large = 16 + (lr * np.float32(16)).astype(np.int32)
        return np.where(n < 16, n, np.minimum(large, 31))
    tab = np.zeros((33, 2, 255), np.float32)
    for m in range(255):
        d0 = 127 - m
        if d0 >= 0:
            tab[int(bucket(np.int32(d0))), 0, m] += 1
            tab[31, 0, m] -= 1
        else:
            tab[32, 0, m] = -30000.0
        d1 = 255 - m
        tab[int(bucket(np.int32(d1))), 1, m] += 1
        tab[31, 1, m] -= 1
    return tab

def attn_consts(inputs, r):
    d = {}
    eye = np.eye(128, dtype=np.float32)
    d["I1b"] = (eye * (r == 1)).astype(BF)
    d["negIb"] = (-30000.0 * eye).astype(BF)
    d["negI0b"] = (-30000.0 * eye * (r == 0)).astype(BF)
    kk = np.arange(1024)[None, None, :]
    j = np.arange(4)[:, None, None]
    q = np.arange(128)[None, :, None]
    own_ok = (kk < 512) & (kk <= 128 * j + q)
    partner_ok = (kk >= 512) & (r == 1)
    d["CM"] = np.where(own_ok | partner_ok, 0.0, -1e30).astype(np.float32).astype(BF)
    d["t5tab"] = t5_tab()
    d["dng_col"] = np.ascontiguousarray(np.asarray(inputs["diff_norm_g"], np.float32)[0].reshape(128, 1))
    return d


from concourse.bass_utils import run_bass_kernel_spmd

NCHK = 8
TOKC = NCHK * 512
NTLC = TOKC // 128
W_KEYS = ["ada_w", "ada_b", "norm_g", "ab_w_in", "ab_w_out", "cd_w_in", "cd_w_out", "rel_bias", "diff_lam",
          "moe_wr_g", "moe_br_g", "moe_wr_e", "moe_br_e", "moe_w_gate", "moe_w_up", "moe_w_down"]


def _build(shapes):
    nc = bass.Bass("TRN2", target_bir_lowering=False)
    C = {}
    for name, (shape, dt) in shapes.items():
        C[name] = nc.dram_tensor(name, list(shape), dt, kind="ExternalInput").ap()

    def scratch(name, shape, dt):
        C[name] = nc.dram_tensor(name, list(shape), dt, kind="Internal").ap()
    scratch("modb", [2, 6, 1024], F32)
    scratch("X1", [2 * TOKC, 1024], F32)
    scratch("X2", [2 * TOKC, 1024], F32)
    scratch("X3", [TOKC, 1024], F32)
    scratch("QcT", [8, 64, TOKC], BF16)
    scratch("QiT", [8, 32, TOKC], BF16)
    scratch("QdT", [8, 64, TOKC], BF16)
    scratch("Wi", [128, NTLC, 8], F32)
    scratch("KcT", [2, 8, 64, TOKC], BF16)
    scratch("KiT", [2, 1, 32, TOKC], BF16)
    scratch("KdT", [2, 8, 64, TOKC], BF16)
    scratch("Vc", [2, 8, 128, NTLC, 65], BF16)
    scratch("Vd", [2, 4, 128, NTLC, 128], BF16)
    scratch("OcTd", [8, 64, TOKC], BF16)
    scratch("OdTd", [4, 128, TOKC], BF16)
    nsl = 32 * moe_cap_tiles(2 * TOKC) * 128 + 128
    scratch("Xs", [nsl, 1024], BF16)
    scratch("Ys", [nsl, 1024], BF16)
    C["KcT2"], C["KdT2"], C["Vc2"], C["Vd2"] = C["KcT"], C["KdT"], C["Vc"], C["Vd"]
    C["KiT2"] = C["KiT"].rearrange("r o m t -> r (o m) t")
    C["out"] = nc.dram_tensor("out", [TOKC, 1024], F32, kind="ExternalOutput").ap()
    phase_mod(nc, C, 0, "m0")
    phase_mod(nc, C, 1, "m1")
    phase_l0mix(nc, C, 2 * NCHK, "l0")
    phase_moe_sparse(nc, C, 0, 2 * NCHK, C["X1"], C["X2"], False, "e0", "ecap0")
    phase_l1proj(nc, C, 2 * NCHK, C["X2"], "pj", NOWN=NCHK)
    phase_attn(nc, C, NCHK, "at")
    phase_attn_out(nc, C, NCHK, C["X2"], C["X3"], "ao")
    phase_moe_sparse(nc, C, 1, NCHK, C["X3"], C["out"], True, "e1", "ecap1")
    return nc


def _dt_of(a):
    return BF16 if a.dtype == BF else (F32 if a.dtype == np.float32 else I32)


def kernel(**inputs):
    inputs = {k: np.asarray(v) for k, v in inputs.items()}
    cores = [(b, r) for b in range(4) for r in range(2)]
    shared = {k: inputs[k] for k in W_KEYS}
    shared["final_norm_g"] = np.ascontiguousarray(inputs["final_norm_g"].reshape(1, 1024))
    shared["Ls"] = np.triu(np.ones((128, 128), np.float32), 1)
    shared["ecap0"] = (np.arange(32, dtype=np.float32) * (moe_cap_tiles(2 * TOKC) * 128))[None]
    shared["ecap1"] = (np.arange(32, dtype=np.float32) * (moe_cap_tiles(TOKC) * 128))[None]
    maps = []
    for (b, r) in cores:
        d = prep_l0(inputs, b, r, NCHK, dup=True)
        d.update(core_consts())
        d.update(attn_consts(inputs, r))
        d.update(shared)
        maps.append(d)
    nc = _build({k: (v.shape, _dt_of(v)) for k, v in maps[0].items()})
    res = run_bass_kernel_spmd(nc, maps, core_ids=list(range(8))).results
    out = np.empty((4, 8192, 1024), np.float32)
    for ci, (b, r) in enumerate(cores):
        o = np.asarray(res[ci]["out"])
        for i in range(NCHK):
            g = 2 * i + r
            out[b, 512 * g:512 * (g + 1)] = o[512 * i:512 * (i + 1)]
    return out
```
